# Optimizing a Trainium2 kernel written in Bass

```python
import jax
import jax.numpy as jnp
from jax import lax
import numpy as np

D_MODEL = 2048
BATCH = 4
SEQ = 4096
DEPTH = 2

MLA_HEADS = 8
MLA_NOPE_DIM = 128
MLA_ROPE_DIM = 64
MLA_V_DIM = 128
MLA_Q_RANK = 512
MLA_KV_RANK = 256
DSA_HEADS = 8
DSA_KV_HEADS = 2
DSA_HEAD_DIM = 128
DSA_ROPE_DIM = DSA_HEAD_DIM // 4
IDX_HEADS = 8
IDX_DIM = 64
IDX_ROPE_DIM = IDX_DIM // 4
TOPK_MAX = 256
CONV_WIDTH = 1024
CONV_K = 3
ROPE_THETA = 500000.0
D_FF_DENSE = 5632
N_EXPERTS = 8
TOP_K = 2
D_FF_EXPERT = 7168
Q_BLOCK = 128
MOE_BLOCK = 128
N_BRANCH = 3
ALPHA = (2 * DEPTH) ** 0.25
BETA = (8 * DEPTH) ** -0.25
LN_EPS = 1e-5
RMS_EPS = 1e-6
N_DENSE_LAYERS = (DEPTH + 1) // 2
N_MOE_LAYERS = DEPTH // 2
IN_SPLIT_SIZES = (MLA_Q_RANK, MLA_KV_RANK, MLA_ROPE_DIM,
                  DSA_HEADS * DSA_HEAD_DIM, DSA_KV_HEADS * DSA_HEAD_DIM, DSA_KV_HEADS * DSA_HEAD_DIM,
                  IDX_HEADS * IDX_DIM, IDX_DIM, IDX_HEADS,
                  CONV_WIDTH, CONV_WIDTH, CONV_WIDTH,
                  N_BRANCH * D_MODEL)
N_IN = sum(IN_SPLIT_SIZES)

kernel_name = "hybrid_mla_dsa_conv_moe_block"


def layer_norm(x, g, b):
    xf = x.astype(jnp.float32)
    mu = jnp.mean(xf, -1, keepdims=True)
    var = jnp.mean(jnp.square(xf - mu), -1, keepdims=True)
    y = (xf - mu) * lax.rsqrt(var + LN_EPS) * g.astype(jnp.float32) + b.astype(jnp.float32)
    return y.astype(x.dtype)


def rms_norm(x, g):
    xf = x.astype(jnp.float32)
    y = xf * lax.rsqrt(jnp.mean(jnp.square(xf), -1, keepdims=True) + RMS_EPS) * g.astype(jnp.float32)
    return y.astype(x.dtype)


def rope_tables(positions, rot_dim):
    inv = ROPE_THETA ** (-jnp.arange(0, rot_dim, 2, dtype=jnp.float32) / rot_dim)
    ang = positions.astype(jnp.float32)[..., None] * inv
    return jnp.cos(ang)[:, :, None, :], jnp.sin(ang)[:, :, None, :]


def apply_rope(x, cos, sin):
    half = cos.shape[-1]
    x1 = x[..., :half].astype(jnp.float32)
    x2 = x[..., half:2 * half].astype(jnp.float32)
    rot = jnp.concatenate([x1 * cos - x2 * sin, x2 * cos + x1 * sin], -1).astype(x.dtype)
    return jnp.concatenate([rot, x[..., 2 * half:]], -1)


def to_blocks(a):
    b, s = a.shape[:2]
    return jnp.moveaxis(a.reshape(b, s // Q_BLOCK, Q_BLOCK, *a.shape[2:]), 1, 0)


def from_blocks(a):
    nb, b, qb = a.shape[:3]
    return jnp.moveaxis(a, 0, 1).reshape(b, nb * qb, *a.shape[3:])


def mla_attention(q, k, v):
    s_len, dq = q.shape[1], q.shape[-1]
    scale = dq ** -0.5
    kpos = jnp.arange(s_len)

    def block(args):
        qb, start = args
        qpos = start + jnp.arange(Q_BLOCK)
        s = jnp.einsum('bqhd,bkhd->bhqk', qb, k).astype(jnp.float32) * scale
        s = jnp.where(kpos[None, :] <= qpos[:, None], s, -jnp.inf)
        p = jax.nn.softmax(s, axis=-1).astype(v.dtype)
        return jnp.einsum('bhqk,bkhd->bqhd', p, v)

    starts = jnp.arange(s_len // Q_BLOCK, dtype=jnp.int32) * Q_BLOCK
    return from_blocks(lax.map(block, (to_blocks(q), starts)))


def dsa_attention(q, k, v, qi, ki, wi, n_sel):
    bsz, s_len, n_h, hd = q.shape
    n_g = k.shape[2]
    n_r = n_h // n_g
    kpos = jnp.arange(s_len)
    gather = jax.vmap(lambda kk, ii: kk[ii])

    def block(args):
        qb, qib, wib, start = args
        qpos = start + jnp.arange(Q_BLOCK)
        logits = jnp.einsum('bqhd,bsd->bqhs', qib, ki).astype(jnp.float32) * (IDX_DIM ** -0.5)
        score = jnp.einsum('bqhs,bqh->bqs', jax.nn.relu(logits), wib.astype(jnp.float32))
        score = jnp.where((kpos[None, :] <= qpos[:, None])[None], score, -jnp.inf)
        _, sel = lax.top_k(score, n_sel)
        kb = gather(k, sel)
        vb = gather(v, sel)
        qg = qb.reshape(bsz, Q_BLOCK, n_g, n_r, hd)
        s = jnp.einsum('bqgrd,bqkgd->bgrqk', qg, kb).astype(jnp.float32) * (hd ** -0.5)
        valid = sel <= qpos[None, :, None]
        s = jnp.where(valid[:, None, None], s, -jnp.inf)
        p = jax.nn.softmax(s, axis=-1).astype(vb.dtype)
        o = jnp.einsum('bgrqk,bqkgd->bqgrd', p, vb)
        return o.reshape(bsz, Q_BLOCK, n_h, hd)

    starts = jnp.arange(s_len // Q_BLOCK, dtype=jnp.int32) * Q_BLOCK
    return from_blocks(lax.map(block, (to_blocks(q), to_blocks(qi), to_blocks(wi), starts)))


def short_conv(u, w, b):
    ch = u.shape[-1]
    y = lax.conv_general_dilated(u, w[:, None, :].astype(u.dtype), window_strides=(1,),
                                 padding=[(CONV_K - 1, 0)], dimension_numbers=('NWC', 'WIO', 'NWC'),
                                 feature_group_count=ch)
    return y + b.astype(u.dtype)


def token_mixer(h, rope_mla, rope_dsa, rope_idx, w_in, q_norm, kv_norm, w_uq, w_ukv, conv_w, conv_b,
                w_br_a, w_br_b, w_br_c, w_o):
    bsz, s_len, d = h.shape
    z = h @ w_in
    split_points = np.cumsum(IN_SPLIT_SIZES)[:-1].tolist()
    (cq, ckv, kr, q, k, v, qi, ki, wi, gb, gc, u, gates) = jnp.split(z, split_points, axis=-1)

    qa = (rms_norm(cq, q_norm) @ w_uq).reshape(bsz, s_len, MLA_HEADS, MLA_NOPE_DIM + MLA_ROPE_DIM)
    qa = jnp.concatenate([qa[..., :MLA_NOPE_DIM], apply_rope(qa[..., MLA_NOPE_DIM:], *rope_mla)], -1)
    kva = (rms_norm(ckv, kv_norm) @ w_ukv).reshape(bsz, s_len, MLA_HEADS, MLA_NOPE_DIM + MLA_V_DIM)
    k_rope = apply_rope(kr[:, :, None, :], *rope_mla)
    ka = jnp.concatenate([kva[..., :MLA_NOPE_DIM],
                          jnp.broadcast_to(k_rope, (bsz, s_len, MLA_HEADS, MLA_ROPE_DIM))], -1)
    ya = mla_attention(qa, ka, kva[..., MLA_NOPE_DIM:]).reshape(bsz, s_len, MLA_HEADS * MLA_V_DIM)

    qb = apply_rope(q.reshape(bsz, s_len, DSA_HEADS, DSA_HEAD_DIM), *rope_dsa)
    kb = apply_rope(k.reshape(bsz, s_len, DSA_KV_HEADS, DSA_HEAD_DIM), *rope_dsa)
    vb = v.reshape(bsz, s_len, DSA_KV_HEADS, DSA_HEAD_DIM)
    qi = apply_rope(qi.reshape(bsz, s_len, IDX_HEADS, IDX_DIM), *rope_idx)
    ki = apply_rope(ki[:, :, None, :], *rope_idx)[:, :, 0]
    n_sel = min(TOPK_MAX, s_len // 4)
    yb = dsa_attention(qb, kb, vb, qi, ki, wi * (IDX_HEADS ** -0.5), n_sel)
    yb = yb.reshape(bsz, s_len, DSA_HEADS * DSA_HEAD_DIM)

    yc = gb * short_conv(gc * u, conv_w, conv_b)

    g = jax.nn.sigmoid(gates.astype(jnp.float32)).astype(h.dtype).reshape(bsz, s_len, N_BRANCH, d)
    merged = g[:, :, 0] * (ya @ w_br_a) + g[:, :, 1] * (yb @ w_br_b) + g[:, :, 2] * (yc @ w_br_c)
    return merged @ w_o


def swiglu(h, w_gate, w_up, w_down):
    return (jax.nn.silu(h @ w_gate) * (h @ w_up)) @ w_down


def moe_swiglu(h, w_router, w_gate, w_up, w_down):
    bsz, s_len, d = h.shape
    xt = h.reshape(-1, d)
    n_tok = xt.shape[0]
    logits = (xt @ w_router).astype(jnp.float32)
    top_logit, top_e = lax.top_k(logits, TOP_K)
    top_g = jax.nn.softmax(top_logit, axis=-1)
    flat_e = top_e.reshape(-1).astype(jnp.int32)
    flat_tok = jnp.repeat(jnp.arange(n_tok, dtype=jnp.int32), TOP_K)
    flat_g = top_g.reshape(-1)
    order = jnp.argsort(flat_e)
    se = flat_e[order]
    counts = jnp.zeros(N_EXPERTS, jnp.int32).at[flat_e].add(1)
    padded = (counts + MOE_BLOCK - 1) // MOE_BLOCK * MOE_BLOCK
    start = jnp.cumsum(counts) - counts
    pstart = jnp.cumsum(padded) - padded
    pend = pstart + padded
    dest = pstart[se] + (jnp.arange(n_tok * TOP_K, dtype=jnp.int32) - start[se])
    n_rows = n_tok * TOP_K + N_EXPERTS * MOE_BLOCK
    n_blocks = n_rows // MOE_BLOCK
    row_tok = jnp.zeros(n_rows, jnp.int32).at[dest].set(flat_tok[order])
    row_gate = jnp.zeros(n_rows, jnp.float32).at[dest].set(flat_g[order])
    blk_start = jnp.arange(n_blocks, dtype=jnp.int32) * MOE_BLOCK
    blk_e = jnp.minimum(jnp.searchsorted(pend, blk_start, side='right'), N_EXPERTS - 1)

    def block(args):
        tok, e = args
        xb = xt[tok]
        return (jax.nn.silu(xb @ w_gate[e]) * (xb @ w_up[e])) @ w_down[e]

    yb = lax.map(block, (row_tok.reshape(n_blocks, MOE_BLOCK), blk_e)).reshape(n_rows, d)
    yb = yb * row_gate[:, None].astype(yb.dtype)
    y = jnp.zeros_like(xt).at[row_tok].add(yb)
    return y.reshape(bsz, s_len, d)


def setup_inputs(seed: int = 0) -> dict:
    key = jax.random.key(seed)
    ks = iter(jax.random.split(key, 40))
    f32 = jnp.float32

    def nrm(shape, scale):
        return jax.random.normal(next(ks), shape, f32) * scale

    def gain(shape):
        return 1.0 + nrm(shape, 0.02)

    d = D_MODEL
    x = nrm((BATCH, SEQ, d), 1.0)
    c = nrm((BATCH, d), 1.0)
    offs = jax.random.randint(next(ks), (BATCH, 1), 0, 1024, dtype=jnp.int32)
    positions = (offs + jnp.arange(SEQ, dtype=jnp.int32)[None, :]).astype(jnp.int32)
    return {
        'x': x,
        'c': c,
        'positions': positions,
        'ada_w': nrm((DEPTH, d, 6 * d), 0.5 * d ** -0.5),
        'ada_b': nrm((DEPTH, 6 * d), 0.02),
        'ln1_g': gain((DEPTH, d)),
        'ln1_b': nrm((DEPTH, d), 0.02),
        'ln2_g': gain((DEPTH, d)),
        'ln2_b': nrm((DEPTH, d), 0.02),
        'w_in': nrm((DEPTH, d, N_IN), d ** -0.5),
        'mla_q_norm': gain((DEPTH, MLA_Q_RANK)),
        'mla_kv_norm': gain((DEPTH, MLA_KV_RANK)),
        'w_uq': nrm((DEPTH, MLA_Q_RANK, MLA_HEADS * (MLA_NOPE_DIM + MLA_ROPE_DIM)), MLA_Q_RANK ** -0.5),
        'w_ukv': nrm((DEPTH, MLA_KV_RANK, MLA_HEADS * (MLA_NOPE_DIM + MLA_V_DIM)), MLA_KV_RANK ** -0.5),
        'conv_w': nrm((DEPTH, CONV_K, CONV_WIDTH), CONV_K ** -0.5),
        'conv_b': nrm((DEPTH, CONV_WIDTH), 0.02),
        'w_branch_a': nrm((DEPTH, MLA_HEADS * MLA_V_DIM, d), (MLA_HEADS * MLA_V_DIM) ** -0.5),
        'w_branch_b': nrm((DEPTH, DSA_HEADS * DSA_HEAD_DIM, d), (DSA_HEADS * DSA_HEAD_DIM) ** -0.5),
        'w_branch_c': nrm((DEPTH, CONV_WIDTH, d), CONV_WIDTH ** -0.5),
        'w_o': nrm((DEPTH, d, d), BETA * d ** -0.5),
        'ffn_w_gate': nrm((N_DENSE_LAYERS, d, D_FF_DENSE), d ** -0.5),
        'ffn_w_up': nrm((N_DENSE_LAYERS, d, D_FF_DENSE), d ** -0.5),
        'ffn_w_down': nrm((N_DENSE_LAYERS, D_FF_DENSE, d), BETA * D_FF_DENSE ** -0.5),
        'router_w': nrm((N_MOE_LAYERS, d, N_EXPERTS), d ** -0.5),
        'moe_w_gate': nrm((N_MOE_LAYERS, N_EXPERTS, d, D_FF_EXPERT), d ** -0.5),
        'moe_w_up': nrm((N_MOE_LAYERS, N_EXPERTS, d, D_FF_EXPERT), d ** -0.5),
        'moe_w_down': nrm((N_MOE_LAYERS, N_EXPERTS, D_FF_EXPERT, d), BETA * D_FF_EXPERT ** -0.5),
    }


def reference(x, c, positions, ada_w, ada_b, ln1_g, ln1_b, ln2_g, ln2_b, w_in, mla_q_norm, mla_kv_norm,
              w_uq, w_ukv, conv_w, conv_b, w_branch_a, w_branch_b, w_branch_c, w_o,
              ffn_w_gate, ffn_w_up, ffn_w_down, router_w, moe_w_gate, moe_w_up, moe_w_down):
    rope_mla = rope_tables(positions, MLA_ROPE_DIM)
    rope_dsa = rope_tables(positions, DSA_ROPE_DIM)
    rope_idx = rope_tables(positions, IDX_ROPE_DIM)
    c_act = jax.nn.silu(c)
    for i in range(DEPTH):
        mod = (c_act @ ada_w[i] + ada_b[i])[:, None, :]
        sh_m, sc_m, g_m, sh_f, sc_f, g_f = jnp.split(mod, 6, axis=-1)
        h = x * (1.0 + sc_m) + sh_m
        mix = token_mixer(h, rope_mla, rope_dsa, rope_idx, w_in[i], mla_q_norm[i], mla_kv_norm[i],
                          w_uq[i], w_ukv[i], conv_w[i], conv_b[i],
                          w_branch_a[i], w_branch_b[i], w_branch_c[i], w_o[i])
        x = layer_norm(ALPHA * x + g_m * mix, ln1_g[i], ln1_b[i])
        h = x * (1.0 + sc_f) + sh_f
        if i % 2 == 0:
            j = i // 2
            f = swiglu(h, ffn_w_gate[j], ffn_w_up[j], ffn_w_down[j])
        else:
            j = i // 2
            f = moe_swiglu(h, router_w[j], moe_w_gate[j], moe_w_up[j], moe_w_down[j])
        x = layer_norm(ALPHA * x + g_f * f, ln2_g[i], ln2_b[i])
    return x
```

```python
import math
from contextlib import ExitStack
import numpy as np
import concourse.bass as bass
import concourse.mybir as mybir
from concourse.bass_utils import run_bass_kernel_spmd

F32 = mybir.dt.float32
BF16 = mybir.dt.bfloat16
I32 = mybir.dt.int32
AF = mybir.ActivationFunctionType
ALU = mybir.AluOpType

D = 2048
S = 4096
NT = S // 128
DEPTH = 2
ALPHA = (2 * DEPTH) ** 0.25
LN_EPS = 1e-5
RMS_EPS = 1e-6
THETA = 500000.0
DFF = 5632
DFE = 7168
NEG = -1.0e30
TWO_PI = 2.0 * math.pi
C_CQ, C_CKV, C_KR, C_Q, C_K, C_V, C_QI, C_KI, C_WI, C_GB, C_GC, C_U, C_G = (
    0, 512, 768, 832, 1856, 2112, 2368, 2880, 2944, 2952, 3976, 5000, 6024)
N_IN = 12168


class Trk:
    def __init__(self, nc, stack, n_dma_sems=56):
        self.nc = nc
        self.eng = {"pe": nc.tensor, "act": nc.scalar, "dve": nc.vector, "pool": nc.gpsimd, "sp": nc.sync}
        self.sem = {}
        self.cnt = {}
        for k in self.eng:
            self.sem[k] = stack.enter_context(nc.semaphore("s_" + k))
            self.cnt[k] = 0
        self.dsem = [stack.enter_context(nc.semaphore("d%d" % i)) for i in range(n_dma_sems)]
        self.dcnt = [0] * n_dma_sems
        self.dnext = 0
        self.known = {k: {} for k in self.eng}
        self.lastw = {}
        self.reads = {}
        self.ninst = 0
        self.nops = 0
        import os
        self.limit = int(os.environ.get("BISECT_N", "0")) or None
        self.reclines = bool(os.environ.get("RECLINES"))
        self.lines = []

    def _wait(self, e, tok):
        s, v, name, owner = tok
        if owner == "pe" and e == "pe":
            return
        kn = self.known[e]
        if kn.get(name, 0) >= v:
            return
        self.eng[e].wait_ge(s, v)
        kn[name] = v
        self.ninst += 1

    def _deps(self, e, reads, writes):
        for k in reads:
            t = self.lastw.get(k)
            if t is not None:
                self._wait(e, t)
        for k in writes:
            t = self.lastw.get(k)
            if t is not None:
                self._wait(e, t)
            for t in self.reads.get(k, ()):
                self._wait(e, t)

    def _commit(self, tok, reads, writes):
        for k in reads:
            self.reads.setdefault(k, []).append(tok)
        for k in writes:
            self.lastw[k] = tok
            self.reads[k] = []

    def _rec(self, e):
        import sys
        f = sys._getframe(2)
        self.lines.append((self.nops, e, f.f_lineno, f.f_back.f_lineno if f.f_back else 0))

    def op(self, e, fn, reads=(), writes=()):
        self.nops += 1
        if self.reclines:
            self._rec(e)
        if self.limit is not None and self.nops > self.limit:
            return None
        self._deps(e, reads, writes)
        ins = fn(self.eng[e])
        self.cnt[e] += 1
        ins.then_inc(self.sem[e], 1)
        tok = (self.sem[e], self.cnt[e], "s_" + e, e)
        self._commit(tok, reads, writes)
        self.ninst += 1
        return tok

    def dma(self, q, fn, reads=(), writes=()):
        self.nops += 1
        if self.reclines:
            self._rec("dma_" + q)
        if self.limit is not None and self.nops > self.limit:
            return None
        i = self.dnext
        self.dnext = (self.dnext + 1) % len(self.dsem)
        name = "d%d" % i
        if self.dcnt[i] > 0:
            self._wait(q, (self.dsem[i], self.dcnt[i], name, "dma"))
        self._deps(q, reads, writes)
        ins = fn(self.eng[q])
        self.dcnt[i] += 16
        ins.then_inc(self.dsem[i], 16)
        tok = (self.dsem[i], self.dcnt[i], name, "dma")
        self._commit(tok, reads, writes)
        self.ninst += 1
        return tok

    def barrier_all(self):
        toks = []
        for k in self.eng:
            if self.cnt[k] > 0:
                toks.append((self.sem[k], self.cnt[k], "s_" + k, k + "_x"))
        for i, s in enumerate(self.dsem):
            if self.dcnt[i] > 0:
                toks.append((s, self.dcnt[i], "d%d" % i, "dma"))
        for e in self.eng:
            for t in toks:
                if e == "pe" and t[2] == "s_pe":
                    continue
                self._wait(e, t)
        self.lastw.clear()
        self.reads.clear()


class StopBuild(Exception):
    pass


def build(dbg=(), stop_after=None):
    nc = bass.Bass("TRN2", target_bir_lowering=False)
    dbg = set(dbg)
    cur = {"L": 0, "ch": -1}

    def chk_stop(phase):
        if stop_after is None:
            return
        tgt = stop_after.split(":")
        if len(tgt) == 3 and int(tgt[0]) == cur["L"] and int(tgt[1]) == cur["ch"] and tgt[2] == phase:
            raise StopBuild()

    def din(name, shape, dt=F32):
        return nc.dram_tensor(name, list(shape), dt, kind="ExternalInput").ap()

    def dscr(name, shape, dt):
        kind = "ExternalOutput" if name in dbg else "Internal"
        return nc.dram_tensor(name, list(shape), dt, kind=kind).ap()

    xb = din("xb", [S, D])
    c_col = din("c_col", [128, 16])
    pos_col = din("pos_col", [128, NT], I32)
    ada_w = din("ada_w", [DEPTH, D, 6 * D])
    ada_b = din("ada_b", [DEPTH, 6 * D])
    ln1_g = din("ln1_g", [DEPTH, D]); ln1_b = din("ln1_b", [DEPTH, D])
    ln2_g = din("ln2_g", [DEPTH, D]); ln2_b = din("ln2_b", [DEPTH, D])
    w_in = din("w_in", [DEPTH, D, N_IN])
    q_norm = din("mla_q_norm", [DEPTH, 512]); kv_norm = din("mla_kv_norm", [DEPTH, 256])
    w_uq = din("w_uq", [DEPTH, 512, 1536]); w_ukv = din("w_ukv", [DEPTH, 256, 2048])
    conv_w = din("conv_w", [DEPTH, 3, 1024]); conv_b = din("conv_b", [DEPTH, 1024])
    w_br = [din("w_branch_a", [DEPTH, 1024, D]), din("w_branch_b", [DEPTH, 1024, D]), din("w_branch_c", [DEPTH, 1024, D])]
    w_o = din("w_o", [DEPTH, D, D])
    ffn_wg = din("ffn_w_gate", [1, D, DFF]); ffn_wu = din("ffn_w_up", [1, D, DFF]); ffn_wd = din("ffn_w_down", [1, DFF, D])
    router_w = din("router_w", [1, D, 8])
    lite = stop_after is not None and not stop_after.endswith(":F") or (stop_after is not None and stop_after.startswith("0:"))
    if lite:
        moe_wg = din("moe_w_gate", [1, 8, 128, 128]); moe_wu = din("moe_w_up", [1, 8, 128, 128]); moe_wd = din("moe_w_down", [1, 8, 128, 128])
    else:
        moe_wg = din("moe_w_gate", [1, 8, D, DFE]); moe_wu = din("moe_w_up", [1, 8, D, DFE]); moe_wd = din("moe_w_down", [1, 8, DFE, D])
    k_ident = din("k_ident", [128, 128])
    k_posrow = din("k_posrow", [128, S])
    k_poscol = din("k_poscol", [128, NT])
    k_inv = din("k_inv", [128, 56])
    q2_posrow = din("q2_posrow", [128, 2048])
    q2_poscol = din("q2_poscol", [128, 16])
    rows1 = din("rows1", [3, 128, NT], I32)
    rows2 = din("rows2", [3, 128, 16], I32)
    out = nc.dram_tensor("out", [2048, D], F32, kind="ExternalOutput").ap()

    ROPE = dscr("ROPE", [S, 112], F32)
    MODBC = dscr("MODBC", [DEPTH, 128, 6 * D], F32)
    X1 = dscr("X1", [S, D], F32)
    KnT = dscr("KnT", [8, 128, S], BF16)
    KrT = dscr("KrT", [64, S], BF16)
    KiT = dscr("KiT", [64, S], BF16)
    Vm = dscr("Vm", [S, 1024], BF16)
    KdT = dscr("KdT", [2, 128, S], BF16)
    Vd = dscr("Vd", [S, 256], BF16)
    GCU = dscr("GCU", [S + 2, 1024], BF16)
    Zcq = dscr("Zcq", [2048, 512], F32)
    Zq = dscr("Zq", [2048, 1024], F32)
    Zqi = dscr("Zqi", [2048, 512], F32)
    Zwi = dscr("Zwi", [2048, 8], F32)
    Zgb = dscr("Zgb", [2048, 1024], BF16)
    Zg = dscr("Zg", [2048, 6144], BF16)
    QnT = dscr("QnT", [8, 128, 2048], BF16)
    QrT = dscr("QrT", [8, 64, 2048], BF16)
    QdT = dscr("QdT", [8, 128, 2048], BF16)
    QiT = dscr("QiT", [8, 64, 2048], BF16)
    YT = [dscr("YaT", [1024, 2048], BF16), dscr("YbT", [1024, 2048], BF16), dscr("YcT", [1024, 2048], BF16)]
    XMID = dscr("XMID", [2048, D], F32)
    H2T = dscr("H2T", [D, 2048], BF16)
    GATE = dscr("GATE", [2048, 8], F32)
    MT = dscr("MT", [D, 2048], BF16)
    MIX = dscr("MIX", [2048, D], F32)
    FOUT = dscr("FOUT", [2048, D], F32)

    with ExitStack() as glob:
        T = Trk(nc, glob)

        uid = [0]

        def sbt(st, name, shape, dt):
            uid[0] += 1
            return st.enter_context(nc.sbuf_tensor("%s_u%d" % (name, uid[0]), list(shape), dt))

        def pst(st, name, shape, dt=F32):
            uid[0] += 1
            return st.enter_context(nc.psum_tensor("%s_u%d" % (name, uid[0]), list(shape), dt))

        def mm(o, l, r, start, stop, rd, wr):
            T.op("pe", lambda e: e.matmul(o, lhsT=l, rhs=r, start=start, stop=stop), rd, wr)

        def tp(o, i, ident, rd, wr):
            T.op("pe", lambda e: e.transpose(o, i, ident), rd, wr)

        def act(o, i, func, rd, wr, **kw):
            T.op("act", lambda e: e.activation(out=o, in_=i, func=func, **kw), rd, wr)

        def ld(q, o, i, rd, wr):
            wr = [k for k in wr if not (isinstance(k, str) and k[0].isupper() and k.isupper() or k in ("KnT", "KrT", "KiT", "Vm", "KdT", "Vd", "QnT", "QrT", "QdT", "QiT", "Y", "GCUpad"))]
            T.dma(q, lambda e: e.dma_start(out=o, in_=i), rd, wr)

        def gather(o, src, idx_ap, rd, wr):
            T.dma("pool", lambda e: e.indirect_dma_start(
                out=o, out_offset=None, in_=src,
                in_offset=bass.IndirectOffsetOnAxis(ap=idx_ap, axis=0)), rd, wr)

        identf = sbt(glob, "identf", [128, 128], F32)
        identb = sbt(glob, "identb", [128, 128], BF16)
        onesb = sbt(glob, "onesb", [128, 128], BF16)
        poscol = sbt(glob, "poscol", [128, NT], F32)
        q2pc = sbt(glob, "q2pc", [128, 16], F32)
        rw1 = sbt(glob, "rw1", [128, 3, NT], I32)
        rw2 = sbt(glob, "rw2", [128, 3, 16], I32)
        modT = sbt(glob, "modT", [128, DEPTH, 96], F32)
        ld("sp", identf[:], k_ident[:, :], [], ["identf"])
        ld("sp", poscol[:], k_poscol[:, :], [], ["poscol"])
        ld("sp", q2pc[:], q2_poscol[:, :], [], ["q2pc"])
        for j in range(3):
            ld("sp", rw1[:, j, :], rows1[j], [], ["rw1"])
            ld("sp", rw2[:, j, :], rows2[j], [], ["rw2"])
        T.op("dve", lambda e: e.tensor_copy(out=identb[:], in_=identf[:]), ["identf"], ["identb"])
        T.op("dve", lambda e: e.memset(onesb[:], 1.0), [], ["onesb"])

        with ExitStack() as st:
            posi = sbt(st, "posi", [128, NT], I32)
            posf = sbt(st, "posf", [128, NT], F32)
            inv = sbt(st, "inv", [128, 56], F32)
            ang = sbt(st, "ang", [128, NT, 56], F32)
            a2 = sbt(st, "a2", [128, NT, 56], F32)
            tab = sbt(st, "tab", [128, NT, 112], F32)
            ld("sp", posi[:], pos_col[:, :], [], ["posi"])
            ld("sp", inv[:], k_inv[:, :], [], ["inv"])
            T.op("dve", lambda e: e.tensor_copy(out=posf[:], in_=posi[:]), ["posi"], ["posf"])
            for t in range(NT):
                T.op("dve", lambda e, t=t: e.tensor_scalar(out=ang[:, t, :], in0=inv[:], scalar1=posf[:, t:t + 1],
                                                           scalar2=None, op0=ALU.mult), ["posf", "inv"], ["ang"])
            ki = sbt(st, "kint", [128, NT, 56], I32)
            kf = sbt(st, "kflt", [128, NT, 56], F32)
            C1 = 6.28125
            C2 = TWO_PI - C1

            def reduce_sin(shift, dst, kd):
                if shift != 0.0:
                    T.op("dve", lambda e: e.tensor_scalar(out=a2[:], in0=ang[:], scalar1=shift, scalar2=None, op0=ALU.add), ["ang", "tabs"], ["a2"])
                    src = a2
                else:
                    src = ang
                T.op("dve", lambda e: e.tensor_scalar(out=kf[:], in0=src[:], scalar1=1.0 / TWO_PI, scalar2=None, op0=ALU.mult), ["ang", "a2"], ["kflt"])
                T.op("dve", lambda e: e.tensor_copy(out=ki[:], in_=kf[:]), ["kflt"], ["kint"])
                T.op("dve", lambda e: e.tensor_copy(out=kf[:], in_=ki[:]), ["kint"], ["kflt"])
                T.op("dve", lambda e: e.scalar_tensor_tensor(out=a2[:], in0=kf[:], scalar=-C1, in1=src[:], op0=ALU.mult, op1=ALU.add), ["kflt", "ang", "a2"], ["a2"])
                T.op("dve", lambda e: e.scalar_tensor_tensor(out=a2[:], in0=kf[:], scalar=-C2, in1=a2[:], op0=ALU.mult, op1=ALU.add), ["kflt", "a2"], ["a2"])
                T.op("dve", lambda e: e.tensor_scalar(out=kf[:], in0=a2[:], scalar1=math.pi, scalar2=-TWO_PI, op0=ALU.is_gt, op1=ALU.mult), ["a2"], ["kflt"])
                T.op("dve", lambda e: e.tensor_tensor(out=a2[:], in0=a2[:], in1=kf[:], op=ALU.add), ["a2", "kflt"], ["a2"])
                T.op("dve", lambda e: e.tensor_scalar(out=kf[:], in0=a2[:], scalar1=-math.pi, scalar2=TWO_PI, op0=ALU.is_lt, op1=ALU.mult), ["a2"], ["kflt"])
                T.op("dve", lambda e: e.tensor_tensor(out=a2[:], in0=a2[:], in1=kf[:], op=ALU.add), ["a2", "kflt"], ["a2"])
                T.op("dve", lambda e: e.tensor_scalar(out=a2[:], in0=a2[:], scalar1=-3.14159, scalar2=3.14159, op0=ALU.max, op1=ALU.min), ["a2"], ["a2"])
                act(dst, a2[:], AF.Sin, ["a2"], [kd])

            reduce_sin(0.0, tab[:, :, 56:112], "tabs")
            reduce_sin(math.pi / 2, tab[:, :, 0:56], "tabc")
            ld("sp", ROPE.rearrange("(t p) f -> p t f", p=128), tab[:], ["tabs", "tabc"], ["ROPE"])
            T.barrier_all()

        def rope(src, dst, cos, sin, H, half, tmp, kin, kout, ktmp):
            x1 = src[:, :, 0:half]; x2 = src[:, :, half:2 * half]
            cb = cos.unsqueeze(1).to_broadcast([128, H, half]); sb_ = sin.unsqueeze(1).to_broadcast([128, H, half])
            t1 = tmp[:, 0, 0:H, 0:half]; t2 = tmp[:, 1, 0:H, 0:half]
            dv = lambda fn, r, w: T.op("dve", fn, r, w)
            dv(lambda e: e.tensor_tensor(out=t1, in0=x1, in1=cb, op=ALU.mult), kin, [ktmp + "1"])
            dv(lambda e: e.tensor_tensor(out=t2, in0=x2, in1=sb_, op=ALU.mult), kin, [ktmp + "2"])
            dv(lambda e: e.tensor_tensor(out=dst[:, :, 0:half], in0=t1, in1=t2, op=ALU.subtract), [ktmp + "1", ktmp + "2"], kout)
            dv(lambda e: e.tensor_tensor(out=t1, in0=x2, in1=cb, op=ALU.mult), kin, [ktmp + "1"])
            dv(lambda e: e.tensor_tensor(out=t2, in0=x1, in1=sb_, op=ALU.mult), kin, [ktmp + "2"])
            dv(lambda e: e.tensor_tensor(out=dst[:, :, half:2 * half], in0=t1, in1=t2, op=ALU.add), [ktmp + "1", ktmp + "2"], kout)
            Dh = src.shape[2]
            if Dh > 2 * half:
                dv(lambda e: e.tensor_copy(out=dst[:, :, 2 * half:Dh], in_=src[:, :, 2 * half:Dh]), kin, kout)

        def layernorm_tile(st_tiles, y_ps_list, xres, gbc, lng, lnb, dst, keys):
            tt, stats, mv, rstd = st_tiles
            kx, ky, kd = keys
            for j in range(4):
                sl = slice(j * 512, (j + 1) * 512)
                T.op("dve", lambda e, j=j, sl=sl: e.tensor_tensor(out=tt[:, sl], in0=y_ps_list[j][0], in1=gbc[:, sl], op=ALU.mult),
                     [y_ps_list[j][1], "gbc"], ["ln_tt%d" % j])
                T.op("dve", lambda e, sl=sl: e.scalar_tensor_tensor(out=tt[:, sl], in0=xres[:, sl], scalar=ALPHA, in1=tt[:, sl],
                                                                    op0=ALU.mult, op1=ALU.add), [kx, "ln_tt%d" % j], ["ln_tt%d" % j])
                T.op("dve", lambda e, j=j, sl=sl: e.bn_stats(out=stats[:, j, :], in_=tt[:, sl]), ["ln_tt%d" % j], ["ln_st%d" % j])
            T.op("dve", lambda e: e.bn_aggr(out=mv[:], in_=stats[:].rearrange("p a b -> p (a b)")), ["ln_st%d" % j for j in range(4)], ["ln_mv"])
            T.op("dve", lambda e: e.tensor_scalar(out=rstd[:], in0=mv[:, 1:2], scalar1=LN_EPS, scalar2=None, op0=ALU.add),
                 ["ln_mv"], ["ln_rstd"])
            act(rstd[:], rstd[:], AF.Sqrt, ["ln_rstd"], ["ln_rstd"])
            T.op("dve", lambda e: e.reciprocal(out=rstd[:], in_=rstd[:]), ["ln_rstd"], ["ln_rstd"])
            for j in range(4):
                sl = slice(j * 512, (j + 1) * 512)
                T.op("dve", lambda e, sl=sl: e.tensor_scalar(out=tt[:, sl], in0=tt[:, sl], scalar1=mv[:, 0:1], scalar2=rstd[:, 0:1],
                                                             op0=ALU.subtract, op1=ALU.mult), ["ln_mv", "ln_rstd", "ln_tt%d" % j], ["ln_tt%d" % j])
                T.op("pool", lambda e, sl=sl: e.tensor_tensor(out=tt[:, sl], in0=tt[:, sl], in1=lng[:, sl], op=ALU.mult),
                     ["ln_tt%d" % j, "lng"], ["ln_tt%d" % j])
                T.op("pool", lambda e, sl=sl: e.tensor_tensor(out=dst[:, sl], in0=tt[:, sl], in1=lnb[:, sl], op=ALU.add),
                     ["ln_tt%d" % j, "lnb"], [kd])

        def layer(L, xin, nchunk, rw, qposrow_dram, qpc, xout, moe):
            cur["L"] = L
            cur["ch"] = -1
            winL = w_in[L].rearrange("(c p) n -> p c n", p=128)
            with ExitStack() as st:
                ccol = sbt(st, "ccol", [128, 16], F32)
                cbc = sbt(st, "cbc", [128, 16, 128], F32)
                awb = [sbt(st, "awb%d" % i, [128, 16, 512], F32) for i in range(2)]
                abb = [sbt(st, "abb%d" % i, [128, 512], F32) for i in range(2)]
                mtmp = [sbt(st, "mtmp%d" % i, [128, 512], F32) for i in range(2)]
                pm = [pst(st, "pm%d" % i, [128, 512]) for i in range(2)]
                pt_ = [pst(st, "ptm%d" % i, [128, 512]) for i in range(2)]
                ld("sp", ccol[:], c_col[:, :], [], ["ccol"])
                for kc in range(16):
                    act(cbc[:, kc, :], ccol[:, kc:kc + 1].to_broadcast([128, 128]), AF.Silu, ["ccol"], ["cbc"])
                awv = ada_w[L].rearrange("(c p) n -> p c n", p=128)
                for b in range(24):
                    s_ = b % 2
                    ld("sp", awb[s_][:], awv[:, :, b * 512:(b + 1) * 512], [], ["awb%d" % s_])
                    ld("sp", abb[s_][:], ada_b[L][b * 512:(b + 1) * 512].partition_broadcast(128), [], ["abb%d" % s_])
                    for kc in range(16):
                        mm(pm[s_][:], cbc[:, kc, :], awb[s_][:, kc, :], kc == 0, kc == 15, ["cbc", "awb%d" % s_], ["pm%d" % s_])
                    T.op("dve", lambda e, s_=s_: e.tensor_tensor(out=mtmp[s_][:], in0=pm[s_][:], in1=abb[s_][:], op=ALU.add),
                         ["pm%d" % s_, "abb%d" % s_], ["mtmp%d" % s_])
                    ld("sp", MODBC[L][:, b * 512:(b + 1) * 512], mtmp[s_][:], ["mtmp%d" % s_], ["MODBC"])
                    for j in range(4):
                        tp(pt_[s_][:, j * 128:(j + 1) * 128], mtmp[s_][:, j * 128:(j + 1) * 128], identf[:], ["mtmp%d" % s_, "identf"], ["ptm%d" % s_])
                    col = b * 4
                    v = col // 16
                    addc = 1.0 if v in (1, 4) else 0.0
                    T.op("dve", lambda e, s_=s_, col=col, addc=addc: e.tensor_scalar(
                        out=modT[:, L, col:col + 4], in0=pt_[s_][:].rearrange("p (j c) -> p j c", c=128)[:, :, 0],
                        scalar1=addc, scalar2=None, op0=ALU.add), ["ptm%d" % s_], ["modT"])
                T.barrier_all()
            chk_stop("ada")

            def build_hT(xt_ap, kx, hT_dst, khT, px, vsh, vsc, extra=None):
                for q4 in range(4):
                    b = px[q4 % 2]
                    for j in range(4):
                        kc = q4 * 4 + j
                        tp(b[0][:, j * 128:(j + 1) * 128], xt_ap[:, kc * 128:(kc + 1) * 128], identf[:], [kx, "identf"], [b[1]])
                    for j in range(4):
                        kc = q4 * 4 + j
                        if extra is None:
                            act(hT_dst[:, kc, :], b[0][:, j * 128:(j + 1) * 128], AF.Identity, [b[1], "modT"], [khT],
                                scale=modT[:, L, vsc * 16 + kc:vsc * 16 + kc + 1], bias=modT[:, L, vsh * 16 + kc:vsh * 16 + kc + 1])
                        else:
                            ek = (extra[1], kc)
                            act(extra[0][:, kc, :], b[0][:, j * 128:(j + 1) * 128], AF.Identity, [b[1], "modT"], [ek],
                                scale=modT[:, L, vsc * 16 + kc:vsc * 16 + kc + 1], bias=modT[:, L, vsh * 16 + kc:vsh * 16 + kc + 1])
                            T.op("dve", lambda e, kc=kc: e.tensor_copy(out=hT_dst[:, kc, :], in_=extra[0][:, kc, :]), [ek], [khT])

            with ExitStack() as st:
                Wk = sbt(st, "Wk", [128, 16, 2944], BF16)
                wukv = sbt(st, "wukv", [128, 2, 2048], BF16)
                kvn = sbt(st, "kvn", [128, 256], F32)
                xt = [sbt(st, "xt%d" % i, [128, D], F32) for i in range(2)]
                hT = [sbt(st, "hT%d" % i, [128, 16, 128], BF16) for i in range(2)]
                rp = [sbt(st, "rp%d" % i, [128, 112], F32) for i in range(2)]
                Asb = sbt(st, "Asb", [128, 384], F32)
                Bsb = sbt(st, "Bsb", [128, 512], F32)
                gcs = sbt(st, "gcs", [128, 1024], F32)
                sq = sbt(st, "sq", [128, 256], F32)
                ssq = sbt(st, "ssq", [128, 1], F32)
                rstd = sbt(st, "rstdk", [128, 1], F32)
                ckvn = sbt(st, "ckvn", [128, 256], BF16)
                ckvT = sbt(st, "ckvT", [128, 2, 128], BF16)
                rtmp = sbt(st, "rtmp", [128, 2, 8, 32], F32)
                knT = [sbt(st, "knT%d" % i, [128, 8, 128], BF16) for i in range(2)]
                vt = [sbt(st, "vt%d" % i, [128, 1024], BF16) for i in range(2)]
                krki = sbt(st, "krki", [128, 2, 64], BF16)
                krkiT = [sbt(st, "krkiT%d" % i, [128, 128], BF16) for i in range(2)]
                kd = sbt(st, "kd", [128, 2, 128], BF16)
                kdT = [sbt(st, "kdT%d" % i, [128, 2, 128], BF16) for i in range(2)]
                vd = [sbt(st, "vd%d" % i, [128, 256], BF16) for i in range(2)]
                gcu = [sbt(st, "gcu%d" % i, [128, 1024], BF16) for i in range(2)]
                zpad = sbt(st, "zpad", [2, 1024], BF16)
                pxs = [pst(st, "pxk%d" % i, [128, 512]) for i in range(2)]
                pps = [pst(st, "ppk%d" % i, [128, 512]) for i in range(2)]
                pms = [pst(st, "pmk%d" % i, [128, 512]) for i in range(2)]
                pbf = pst(st, "pbfk", [128, 1024], BF16)
                px = [(pxs[i], "pxk%d" % i) for i in range(2)]
                segs = [(0, C_CKV, 320), (320, C_KI, 64), (384, C_K, 512), (896, C_GC, 2048)]
                for (lo, src, n) in segs:
                    for o in range(0, n, 512):
                        w_ = min(512, n - o)
                        ld("pool", Wk[:, :, lo + o:lo + o + w_], winL[:, :, src + o:src + o + w_], [], ["Wk"])
                ld("pool", wukv[:], w_ukv[L].rearrange("(c p) n -> p c n", p=128), [], ["wukv"])
                ld("sp", kvn[:], kv_norm[L].partition_broadcast(128), [], ["kvn"])
                T.op("dve", lambda e: e.memset(zpad[:], 0.0), [], ["zpad"])
                ld("sp", GCU[0:2, :], zpad[:], ["zpad"], ["GCUpad"])
                groups = [(0, 384), (384, 896), (896, 1408), (1408, 1920), (1920, 2432), (2432, 2944)]
                for t in range(NT):
                    s_ = t % 2
                    r0 = t * 128
                    kx = "xt%d" % s_
                    ld("sp", xt[s_][:], xin[r0:r0 + 128, :], [], [kx])
                    ld("sp", rp[s_][:], ROPE[r0:r0 + 128, :], [], ["rp%d" % s_])
                    khT = "hT%d" % s_
                    build_hT(xt[s_], kx, hT[s_], khT, px, 0, 1)
                    cosm, sinm = rp[s_][:, 0:32], rp[s_][:, 56:88]
                    cosd, sind = rp[s_][:, 32:48], rp[s_][:, 88:104]
                    cosi, sini = rp[s_][:, 48:56], rp[s_][:, 104:112]
                    krp = "rp%d" % s_
                    for gi, (lo, hi) in enumerate(groups):
                        pb = pps[gi % 2]; kp = "ppk%d" % (gi % 2)
                        n = hi - lo
                        for kc in range(16):
                            mm(pb[:, 0:n], hT[s_][:, kc, :], Wk[:, kc, lo:hi], kc == 0, kc == 15, [khT, "Wk"], [kp])
                        if gi == 0:
                            act(Asb[:], pb[:, 0:384], AF.Copy, [kp], ["Asb"])
                            act(sq[:], Asb[:, 0:256], AF.Square, ["Asb"], ["sq", "ssq"], accum_out=ssq[:])
                            T.op("dve", lambda e: e.tensor_scalar(out=rstd[:], in0=ssq[:], scalar1=1.0 / 256, scalar2=RMS_EPS,
                                                                  op0=ALU.mult, op1=ALU.add), ["ssq"], ["rstdk"])
                            act(rstd[:], rstd[:], AF.Sqrt, ["rstdk"], ["rstdk"])
                            T.op("dve", lambda e: e.reciprocal(out=rstd[:], in_=rstd[:]), ["rstdk"], ["rstdk"])
                            T.op("dve", lambda e: e.scalar_tensor_tensor(out=ckvn[:], in0=Asb[:, 0:256], scalar=rstd[:, 0:1], in1=kvn[:],
                                                                         op0=ALU.mult, op1=ALU.mult), ["Asb", "rstdk", "kvn"], ["ckvn"])
                            for c2 in range(2):
                                tp(pbf[:, c2 * 128:(c2 + 1) * 128], ckvn[:, c2 * 128:(c2 + 1) * 128], identb[:], ["ckvn", "identb"], ["pbfk"])
                            act(ckvT[:].rearrange("p a b -> p (a b)"), pbf[:, 0:256], AF.Copy, ["pbfk"], ["ckvT"])
                            for h in range(8):
                                pmb = pms[h // 4]; kpm = "pmk%d" % (h // 4)
                                for c2 in range(2):
                                    mm(pmb[:, (h % 4) * 128:(h % 4 + 1) * 128], wukv[:, c2, h * 256:h * 256 + 128], ckvT[:, c2, :],
                                       c2 == 0, c2 == 1, ["wukv", "ckvT"], [kpm])
                            for hh in range(2):
                                act(knT[s_][:, hh * 4:(hh + 1) * 4, :].rearrange("p a b -> p (a b)"), pms[hh][:], AF.Copy,
                                    ["pmk%d" % hh], ["knT%d" % s_])
                            ld("sp", KnT[:, :, r0:r0 + 128].rearrange("h d t -> d h t"), knT[s_][:], ["knT%d" % s_], ["KnT"])
                            wv = wukv[:].rearrange("p c (h d) -> p c h d", d=256)
                            for hh in range(2):
                                for c2 in range(2):
                                    mm(pms[hh][:], ckvT[:, c2, :], wv[:, c2, hh * 4:(hh + 1) * 4, 128:256], c2 == 0, c2 == 1,
                                       ["ckvT", "wukv"], ["pmk%d" % hh])
                                act(vt[s_][:, hh * 512:(hh + 1) * 512], pms[hh][:], AF.Copy, ["pmk%d" % hh], ["vt%d" % s_])
                            ld("sp", Vm[r0:r0 + 128, :], vt[s_][:], ["vt%d" % s_], ["Vm"])
                            rope(Asb[:, 256:320].rearrange("p (h d) -> p h d", h=1), krki[:, 0:1, :], cosm, sinm, 1, 32, rtmp,
                                 ["Asb", krp], ["krki"], "rtk")
                            rope(Asb[:, 320:384].rearrange("p (h d) -> p h d", h=1), krki[:, 1:2, :], cosi, sini, 1, 8, rtmp,
                                 ["Asb", krp], ["krki"], "rtk")
                            tp(pbf[:, 256:384], krki[:].rearrange("p a b -> p (a b)"), identb[:], ["krki", "identb"], ["pbfk2"])
                            act(krkiT[s_][:], pbf[:, 256:384], AF.Copy, ["pbfk2"], ["krkiT%d" % s_])
                            ld("sp", KrT[:, r0:r0 + 128], krkiT[s_][0:64, :], ["krkiT%d" % s_], ["KrT"])
                            ld("sp", KiT[:, r0:r0 + 128], krkiT[s_][64:128, :], ["krkiT%d" % s_], ["KiT"])
                        elif gi == 1:
                            act(Bsb[:], pb[:, 0:512], AF.Copy, [kp], ["Bsb"])
                            rope(Bsb[:, 0:256].rearrange("p (h d) -> p h d", h=2), kd[:], cosd, sind, 2, 16, rtmp,
                                 ["Bsb", krp], ["kd"], "rtk")
                            for g2 in range(2):
                                tp(pbf[:, 512 + g2 * 128:512 + (g2 + 1) * 128], kd[:, g2, :], identb[:], ["kd", "identb"], ["pbfk3"])
                            act(kdT[s_][:].rearrange("p a b -> p (a b)"), pbf[:, 512:768], AF.Copy, ["pbfk3"], ["kdT%d" % s_])
                            ld("sp", KdT[:, :, r0:r0 + 128].rearrange("g d t -> d g t"), kdT[s_][:], ["kdT%d" % s_], ["KdT"])
                            T.op("dve", lambda e, s_=s_: e.tensor_copy(out=vd[s_][:], in_=Bsb[:, 256:512]), ["Bsb"], ["vd%d" % s_])
                            ld("sp", Vd[r0:r0 + 128, :], vd[s_][:], ["vd%d" % s_], ["Vd"])
                        elif gi in (2, 3):
                            o = (gi - 2) * 512
                            act(gcs[:, o:o + 512], pb[:, 0:512], AF.Copy, [kp], ["gcs%d" % gi])
                        else:
                            o = (gi - 4) * 512
                            T.op("dve", lambda e, o=o, pb=pb, s_=s_: e.tensor_tensor(out=gcu[s_][:, o:o + 512], in0=pb[:, 0:512],
                                                                                in1=gcs[:, o:o + 512], op=ALU.mult),
                                 [kp, "gcs%d" % (gi - 2)], ["gcu%d" % s_])
                    ld("sp", GCU[2 + r0:2 + r0 + 128, :], gcu[s_][:], ["gcu%d" % s_], ["GCU"])
                T.barrier_all()
            chk_stop("K")

            for ch in range(nchunk):
                cur["ch"] = ch
                chunk(L, ch, winL, rw, qposrow_dram, qpc, xin, xout, moe, build_hT)
                chk_stop("F")
            cur["ch"] = -1

        def chunk(L, ch, winL, rw, qposrow_dram, qpc, xin, xout, moe, build_hT):
            tcol = lambda i: ch * 16 + i
            with ExitStack() as st:
                hTa = sbt(st, "hTa", [128, 16, 2048], BF16)
                xt = [sbt(st, "xq%d" % i, [128, D], F32) for i in range(2)]
                wb = [sbt(st, "wqb%d" % i, [128, 16, 512], BF16) for i in range(2)]
                zo = [sbt(st, "zo%d" % i, [128, 512], F32) for i in range(2)]
                zob = [sbt(st, "zob%d" % i, [128, 512], BF16) for i in range(2)]
                pxs = [pst(st, "pxq%d" % i, [128, 512]) for i in range(2)]
                pps = [pst(st, "ppq%d" % i, [128, 512]) for i in range(3)]
                px = [(pxs[i], "pxq%d" % i) for i in range(2)]
                for i in range(16):
                    s_ = i % 2
                    gather(xt[s_][:], xin[:, :], rw[:, 0, tcol(i):tcol(i) + 1], ["rw"], ["xq%d" % s_])
                    build_hT(xt[s_], "xq%d" % s_, hTa[:, :, i * 128:(i + 1) * 128], "hTa", px, 0, 1)
                blocks = [(C_CQ, 512, "cq", 0), (C_Q, 512, "q", 0), (C_Q + 512, 512, "q", 512), (C_QI, 512, "qi", 0),
                          (C_WI, 8, "wi", 0), (C_GB, 512, "gb", 0), (C_GB + 512, 512, "gb", 512)]
                blocks += [(C_G + j * 512, 512, "g", j * 512) for j in range(12)]
                dst = {"cq": Zcq, "q": Zq, "qi": Zqi, "wi": Zwi, "gb": Zgb, "g": Zg}
                cnt = 0
                for bi, (c0, n, kind, o) in enumerate(blocks):
                    s_ = bi % 2
                    ld("pool", wb[s_][:, :, 0:n], winL[:, :, c0:c0 + n], [], ["wqb%d" % s_])
                    for i in range(16):
                        pi = cnt % 3; zi = cnt % 2; cnt += 1
                        pb = pps[pi]; kp = "ppq%d" % pi
                        for kc in range(16):
                            mm(pb[:, 0:n], hTa[:, kc, i * 128:(i + 1) * 128], wb[s_][:, kc, 0:n], kc == 0, kc == 15,
                               ["hTa", "wqb%d" % s_], [kp])
                        rows = slice(i * 128, (i + 1) * 128)
                        if kind in ("g", "gb"):
                            act(zob[zi][:, 0:n], pb[:, 0:n], AF.Sigmoid if kind == "g" else AF.Copy, [kp], ["zob%d" % zi])
                            ld("sp", dst[kind][rows, o:o + n], zob[zi][:, 0:n], ["zob%d" % zi], [("Z", kind, i)])
                        else:
                            act(zo[zi][:, 0:n], pb[:, 0:n], AF.Copy, [kp], ["zo%d" % zi])
                            ld("sp", dst[kind][rows, o:o + n], zo[zi][:, 0:n], ["zo%d" % zi], [("Z", kind, i)])
                T.barrier_all()
            chk_stop("Q2")
            with ExitStack() as st:
                wuq = sbt(st, "wuq", [128, 4, 1536], BF16)
                qn = sbt(st, "qn", [128, 512], F32)
                ld("pool", wuq[:], w_uq[L].rearrange("(c p) n -> p c n", p=128), [], ["wuq"])
                ld("sp", qn[:], q_norm[L].partition_broadcast(128), [], ["qn"])
                cq = [sbt(st, "cq%d" % i, [128, 512], F32) for i in range(2)]
                qq = [sbt(st, "qq%d" % i, [128, 1024], F32) for i in range(2)]
                qi_ = [sbt(st, "qi%d" % i, [128, 512], F32) for i in range(2)]
                rp = [sbt(st, "rq%d" % i, [128, 112], F32) for i in range(2)]
                sq = sbt(st, "sqq", [128, 512], F32)
                ssq = sbt(st, "ssqq", [128, 1], F32)
                rstd = sbt(st, "rstdq", [128, 1], F32)
                cqn = sbt(st, "cqn", [128, 512], BF16)
                cqT = sbt(st, "cqT", [128, 4, 128], BF16)
                qrs = sbt(st, "qrs", [128, 512], F32)
                rtmp = sbt(st, "rtmpq", [128, 2, 8, 32], F32)
                qrb = sbt(st, "qrb", [128, 8, 64], BF16)
                qdb = sbt(st, "qdb", [128, 8, 128], BF16)
                qib = sbt(st, "qib", [128, 8, 64], BF16)
                qnT = [sbt(st, "qnT%d" % i, [128, 8, 128], BF16) for i in range(2)]
                qrT = [sbt(st, "qrT%d" % i, [128, 4, 128], BF16) for i in range(2)]
                qdT = [sbt(st, "qdT%d" % i, [128, 8, 128], BF16) for i in range(2)]
                qiT = [sbt(st, "qiT%d" % i, [128, 4, 128], BF16) for i in range(2)]
                pms = [pst(st, "pm3%d" % i, [128, 512]) for i in range(3)]
                pbfs = [pst(st, "pbf3%d" % i, [128, 1024], BF16) for i in range(2)]
                for i in range(16):
                    s_ = i % 2
                    rows = slice(i * 128, (i + 1) * 128)
                    ld("sp", cq[s_][:], Zcq[rows, :], [], ["cq%d" % s_])
                    ld("sp", qq[s_][:], Zq[rows, :], [], ["qq%d" % s_])
                    ld("sp", qi_[s_][:], Zqi[rows, :], [], ["qi%d" % s_])
                    gather(rp[s_][:], ROPE[:, :], rw[:, 0, tcol(i):tcol(i) + 1], ["rw"], ["rq%d" % s_])
                    krp = "rq%d" % s_
                    cosm, sinm = rp[s_][:, 0:32], rp[s_][:, 56:88]
                    cosd, sind = rp[s_][:, 32:48], rp[s_][:, 88:104]
                    cosi, sini = rp[s_][:, 48:56], rp[s_][:, 104:112]
                    act(sq[:], cq[s_][:], AF.Square, ["cq%d" % s_], ["sqq", "ssqq"], accum_out=ssq[:])
                    T.op("dve", lambda e: e.tensor_scalar(out=rstd[:], in0=ssq[:], scalar1=1.0 / 512, scalar2=RMS_EPS,
                                                          op0=ALU.mult, op1=ALU.add), ["ssqq"], ["rstdq"])
                    act(rstd[:], rstd[:], AF.Sqrt, ["rstdq"], ["rstdq"])
                    T.op("dve", lambda e: e.reciprocal(out=rstd[:], in_=rstd[:]), ["rstdq"], ["rstdq"])
                    T.op("dve", lambda e, s_=s_: e.scalar_tensor_tensor(out=cqn[:], in0=cq[s_][:], scalar=rstd[:, 0:1], in1=qn[:],
                                                                        op0=ALU.mult, op1=ALU.mult), ["cq%d" % s_, "rstdq", "qn"], ["cqn"])
                    for c4 in range(4):
                        tp(pbfs[0][:, c4 * 128:(c4 + 1) * 128], cqn[:, c4 * 128:(c4 + 1) * 128], identb[:], ["cqn", "identb"], ["pbf30a"])
                    act(cqT[:].rearrange("p a b -> p (a b)"), pbfs[0][:, 0:512], AF.Copy, ["pbf30a"], ["cqT"])
                    for h in range(8):
                        pmb = pms[h // 4]; kpm = "pm3%d" % (h // 4)
                        for c4 in range(4):
                            mm(pmb[:, (h % 4) * 128:(h % 4 + 1) * 128], wuq[:, c4, h * 192:h * 192 + 128], cqT[:, c4, :],
                               c4 == 0, c4 == 3, ["wuq", "cqT"], [kpm])
                    for hh in range(2):
                        act(qnT[s_][:, hh * 4:(hh + 1) * 4, :].rearrange("p a b -> p (a b)"), pms[hh][:], AF.Copy, ["pm3%d" % hh], ["qnT%d" % s_])
                    ld("sp", QnT[:, :, rows].rearrange("h d t -> d h t"), qnT[s_][:], ["qnT%d" % s_], ["QnT"])
                    wr_ = wuq[:].rearrange("p c (h d) -> p c h d", d=192)
                    for c4 in range(4):
                        mm(pms[2][:], cqT[:, c4, :], wr_[:, c4, :, 128:192], c4 == 0, c4 == 3, ["cqT", "wuq"], ["pm32"])
                    act(qrs[:], pms[2][:], AF.Copy, ["pm32"], ["qrs"])
                    rope(qrs[:].rearrange("p (h d) -> p h d", h=8), qrb[:], cosm, sinm, 8, 32, rtmp, ["qrs", krp], ["qrb"], "rtq")
                    for j in range(4):
                        tp(pbfs[0][:, 512 + j * 128:512 + (j + 1) * 128], qrb[:, 2 * j:2 * j + 2, :].rearrange("p a b -> p (a b)"), identb[:],
                           ["qrb", "identb"], ["pbf30b"])
                    act(qrT[s_][:].rearrange("p a b -> p (a b)"), pbfs[0][:, 512:1024], AF.Copy, ["pbf30b"], ["qrT%d" % s_])
                    for j in range(4):
                        ld("sp", QrT[2 * j:2 * j + 2, :, rows].rearrange("h d t -> (h d) t"), qrT[s_][:, j, :], ["qrT%d" % s_], ["QrT"])
                    rope(qq[s_][:].rearrange("p (h d) -> p h d", h=8), qdb[:], cosd, sind, 8, 16, rtmp, ["qq%d" % s_, krp], ["qdb"], "rtq")
                    for h in range(8):
                        tp(pbfs[1][:, h * 128:(h + 1) * 128], qdb[:, h, :], identb[:], ["qdb", "identb"], ["pbf31"])
                    act(qdT[s_][:].rearrange("p a b -> p (a b)"), pbfs[1][:], AF.Copy, ["pbf31"], ["qdT%d" % s_])
                    ld("sp", QdT[:, :, rows].rearrange("h d t -> d h t"), qdT[s_][:], ["qdT%d" % s_], ["QdT"])
                    rope(qi_[s_][:].rearrange("p (h d) -> p h d", h=8), qib[:], cosi, sini, 8, 8, rtmp, ["qi%d" % s_, krp], ["qib"], "rtq")
                    for j in range(4):
                        tp(pbfs[0][:, j * 128:(j + 1) * 128], qib[:, 2 * j:2 * j + 2, :].rearrange("p a b -> p (a b)"), identb[:],
                           ["qib", "identb"], ["pbf30a"])
                    act(qiT[s_][:].rearrange("p a b -> p (a b)"), pbfs[0][:, 0:512], AF.Copy, ["pbf30a"], ["qiT%d" % s_])
                    for j in range(4):
                        ld("sp", QiT[2 * j:2 * j + 2, :, rows].rearrange("h d t -> (h d) t"), qiT[s_][:, j, :], ["qiT%d" % s_], ["QiT"])
                T.barrier_all()
            chk_stop("Q3")

            if L == 0:
                kmax = [4 * (4 * ch + g) + 4 for g in range(4)]
                kfull = [4 * (4 * ch + g) for g in range(4)]
            else:
                kmax = [8 * g + 8 for g in range(4)]
                kfull = [8 * g for g in range(4)]

            def attn_core(st, heads, load_head, s_mm, vslice, mask_fn, scale, Ydst):
                pt = [sbt(st, "pt%d" % i, [128, 512], BF16) for i in range(3)]
                rden = sbt(st, "rden", [128, 512], F32)
                yo = [sbt(st, "yo%d" % i, [128, 512], BF16) for i in range(2)]
                pS = [pst(st, "pS%d" % i, [128, 512]) for i in range(3)]
                pO = [pst(st, "pO%d" % i, [128, 512]) for i in range(2)]
                pD = [pst(st, "pD%d" % i, [128, 512]) for i in range(2)]
                it = 0
                gi = 0
                for h in heads:
                    load_head(h)
                    for g in range(4):
                        o_ = gi % 2; gi += 1
                        nk = kmax[g]
                        for j in range(nk):
                            si = it % 3; it += 1
                            s_mm(h, g, j, pS[si], "pS%d" % si)
                            act(pt[si][:], pS[si][:], AF.Exp, ["pS%d" % si], ["pt%d" % si], scale=scale)
                            mk = mask_fn(g, j)
                            if mk is not None:
                                T.op("pool", lambda e, si=si, mk=mk: e.tensor_tensor(out=pt[si][:], in0=pt[si][:], in1=mk[0], op=ALU.mult),
                                     ["pt%d" % si, mk[1]], ["pt%d" % si])
                            va, kv = vslice(h, j)
                            mm(pO[o_][:], va, pt[si][:], j == 0, j == nk - 1, [kv, "pt%d" % si], ["pO%d" % o_])
                            mm(pD[o_][:], onesb[:], pt[si][:], j == 0, j == nk - 1, ["onesb", "pt%d" % si], ["pD%d" % o_])
                        T.op("dve", lambda e, o_=o_: e.reciprocal(out=rden[:], in_=pD[o_][:]), ["pD%d" % o_], ["rden"])
                        T.op("dve", lambda e, o_=o_: e.tensor_tensor(out=yo[o_][:], in0=pO[o_][:], in1=rden[:], op=ALU.mult),
                             ["pO%d" % o_, "rden"], ["yo%d" % o_])
                        ld("sp", Ydst[h * 128:(h + 1) * 128, g * 512:(g + 1) * 512], yo[o_][:], ["yo%d" % o_], ["Y"])

            with ExitStack() as st:
                qpr = sbt(st, "qpr", [128, 2048], F32)
                cm = sbt(st, "cm", [128, 4, 8, 512], BF16)
                krT = sbt(st, "krT", [64, S], BF16)
                knT = [sbt(st, "aknT%d" % i, [128, S], BF16) for i in range(2)]
                vv = [sbt(st, "avv%d" % i, [128, NT, 128], BF16) for i in range(2)]
                qnT = [sbt(st, "aqn%d" % i, [128, 2048], BF16) for i in range(2)]
                qrT = [sbt(st, "aqr%d" % i, [64, 2048], BF16) for i in range(2)]
                ld("sp", qpr[:], qposrow_dram[:, ch * 2048:(ch + 1) * 2048], [], ["qpr"])
                ld("sp", krT[:], KrT[:, :], [], ["krT"])
                nband = kmax[0] - kfull[0]
                for g in range(4):
                    for jj in range(nband):
                        j = kfull[g] + jj
                        T.op("dve", lambda e, g=g, jj=jj, j=j: e.tensor_scalar(out=cm[:, g, jj, :], in0=qpr[:, g * 512:(g + 1) * 512],
                                                                              scalar1=poscol[:, j:j + 1], scalar2=None, op0=ALU.is_ge),
                             ["qpr", "poscol"], ["cm"])
                hs = {}

                def load_head(h):
                    s_ = h % 2
                    hs["s"] = s_
                    ld("sp", knT[s_][:], KnT[h], [], ["aknT%d" % s_])
                    ld("sp", vv[s_][:], Vm[:, h * 128:(h + 1) * 128].rearrange("(j p) d -> p j d", p=128), [], ["avv%d" % s_])
                    ld("sp", qnT[s_][:], QnT[h], [], ["aqn%d" % s_])
                    ld("sp", qrT[s_][:], QrT[h], [], ["aqr%d" % s_])

                def s_mm(h, g, j, ps, kps):
                    s_ = h % 2
                    mm(ps[:], knT[s_][:, j * 128:(j + 1) * 128], qnT[s_][:, g * 512:(g + 1) * 512], True, False,
                       ["aknT%d" % s_, "aqn%d" % s_], [kps])
                    mm(ps[:], krT[:, j * 128:(j + 1) * 128], qrT[s_][:, g * 512:(g + 1) * 512], False, True,
                       ["krT", "aqr%d" % s_], [kps])

                def vslice(h, j):
                    return vv[h % 2][:, j, :], "avv%d" % (h % 2)

                def mask_fn(g, j):
                    if j < kfull[g]:
                        return None
                    return (cm[:, g, j - kfull[g], :], "cm")

                attn_core(st, list(range(8)), load_head, s_mm, vslice, mask_fn, 192.0 ** -0.5, YT[0])
                T.barrier_all()
            chk_stop("A1")

            with ExitStack() as st:
                prow = sbt(st, "prow", [128, S], F32)
                kiT = sbt(st, "kiT", [64, S], BF16)
                qiT = sbt(st, "aqi", [64, 8, 2048], BF16)
                wi = sbt(st, "wi", [128, 16, 8], F32)
                kdT = sbt(st, "akd", [128, 2, S], BF16)
                vdd = sbt(st, "avd", [128, NT, 256], BF16)
                SC = sbt(st, "SC", [128, S], F32)
                WK = sbt(st, "WK", [128, S], F32)
                Rr = [sbt(st, "Rr%d" % i, [128, 512], F32) for i in range(2)]
                bias = sbt(st, "cbias", [128, 1024], F32)
                m8 = sbt(st, "m8", [128, 8], F32)
                thr = sbt(st, "thr", [128, 1], F32)
                Mk = sbt(st, "Mk", [128, S], BF16)
                mT = sbt(st, "mT", [128, 32, 512], BF16)
                qd = [sbt(st, "aqd%d" % i, [128, 512], BF16) for i in range(2)]
                pI = [pst(st, "pI%d" % i, [128, 512]) for i in range(1)]
                ld("sp", prow[:], k_posrow[:, :], [], ["prow"])
                ld("sp", kiT[:], KiT[:, :], [], ["kiT"])
                ld("sp", qiT[:], QiT.rearrange("h d t -> d h t"), [], ["aqi"])
                ld("sp", wi[:], Zwi.rearrange("(i p) h -> p i h", p=128), [], ["wi"])
                ld("sp", kdT[:], KdT.rearrange("g d t -> d g t"), [], ["akd"])
                ld("sp", vdd[:], Vd.rearrange("(j p) d -> p j d", p=128), [], ["avd"])
                pt = [sbt(st, "pt%d" % i, [128, 512], BF16) for i in range(3)]
                rden = sbt(st, "rden", [128, 512], F32)
                yo = [sbt(st, "yo%d" % i, [128, 512], BF16) for i in range(2)]
                pS = [pst(st, "pS%d" % i, [128, 512]) for i in range(3)]
                pO = [pst(st, "pO%d" % i, [128, 512]) for i in range(1)]
                pD = [pst(st, "pD%d" % i, [128, 512]) for i in range(1)]
                pbf = pst(st, "pbfa", [128, 1024], BF16)
                it = 0
                for g in range(4):
                    nk = kmax[g]
                    E = nk * 128
                    band0 = kfull[g] * 128
                    for qi4 in range(4):
                        i = g * 4 + qi4
                        for kb in range(E // 512):
                            cs = slice(kb * 512, (kb + 1) * 512)
                            for hh in range(8):
                                r_ = hh % 2
                                mm(pI[0][:], qiT[:, hh, i * 128:(i + 1) * 128], kiT[:, cs], True, True, ["aqi", "kiT"], ["pI0"])
                                act(Rr[r_][:], pI[0][:], AF.Relu, ["pI0"], ["Rr%d" % r_])
                                if hh == 0:
                                    T.op("dve", lambda e, r_=r_, cs=cs, i=i, hh=hh: e.tensor_scalar(
                                        out=SC[:, cs], in0=Rr[r_][:], scalar1=wi[:, i, hh:hh + 1], scalar2=None, op0=ALU.mult),
                                        ["Rr%d" % r_, "wi"], ["SC"])
                                else:
                                    T.op("dve", lambda e, r_=r_, cs=cs, i=i, hh=hh: e.scalar_tensor_tensor(
                                        out=SC[:, cs], in0=Rr[r_][:], scalar=wi[:, i, hh:hh + 1], in1=SC[:, cs],
                                        op0=ALU.mult, op1=ALU.add), ["Rr%d" % r_, "wi", "SC"], ["SC"])
                        if stop_after == "c0A2a":
                            T.barrier_all()
                            return
                        bw = E - band0
                        T.op("dve", lambda e, i=i, band0=band0, bw=bw: e.tensor_scalar(
                            out=bias[:, 0:bw], in0=prow[:, band0:E], scalar1=qpc[:, tcol(i):tcol(i) + 1], scalar2=NEG,
                            op0=ALU.is_gt, op1=ALU.mult), ["prow"], ["cbias"])
                        T.op("dve", lambda e, band0=band0, bw=bw, E=E: e.tensor_tensor(out=SC[:, band0:E], in0=SC[:, band0:E], in1=bias[:, 0:bw],
                                                                                  op=ALU.add), ["SC", "cbias"], ["SC"])
                        src = SC
                        for r in range(32):
                            T.op("dve", lambda e, src=src, E=E: e.max(out=m8[:], in_=src[:, 0:E]), ["SC", "WK"], ["m8"])
                            if r < 31:
                                T.op("dve", lambda e, src=src, E=E: e.match_replace(out=WK[:, 0:E], in_to_replace=m8[:], in_values=src[:, 0:E],
                                                                                imm_value=NEG), ["m8", "SC", "WK"], ["WK"])
                            src = WK
                        T.op("dve", lambda e: e.tensor_scalar(out=thr[:], in0=m8[:, 7:8], scalar1=-1.0e29, scalar2=None, op0=ALU.max),
                             ["m8"], ["thr"])
                        T.op("dve", lambda e, E=E: e.tensor_scalar(out=Mk[:, 0:E], in0=SC[:, 0:E], scalar1=thr[:, 0:1], scalar2=None,
                                                                   op0=ALU.is_ge), ["SC", "thr"], ["Mk"])
                        for j4 in range(nk // 4):
                            half = 0
                            for jj in range(4):
                                j = j4 * 4 + jj
                                tp(pbf[:, half + jj * 128:half + (jj + 1) * 128], Mk[:, j * 128:(j + 1) * 128], identb[:],
                                   ["Mk", "identb"], ["pbfa0"])
                            for jj in range(4):
                                act(mT[:, j4 * 4 + jj, qi4 * 128:(qi4 + 1) * 128],
                                    pbf[:, half + jj * 128:half + (jj + 1) * 128], AF.Copy, ["pbfa0"], [("mT", j4 * 4 + jj)])
                    if stop_after == "c0A2b" or (stop_after == "c0A2d" and g == 1):
                        T.barrier_all()
                        return
                    for h in range(8):
                        q_ = h % 2
                        ld("sp", qd[q_][:], QdT[h][:, g * 512:(g + 1) * 512], [], ["aqd%d" % q_])
                        for j in range(nk):
                            si = it % 3; it += 1
                            mm(pS[si][:], kdT[:, h // 4, j * 128:(j + 1) * 128], qd[q_][:], True, True, ["akd", "aqd%d" % q_], ["pS%d" % si])
                            act(pt[si][:], pS[si][:], AF.Exp, ["pS%d" % si], ["pt%d" % si], scale=128.0 ** -0.5)
                            T.op("pool", lambda e, si=si, j=j: e.tensor_tensor(out=pt[si][:], in0=pt[si][:], in1=mT[:, j, :], op=ALU.mult),
                                 ["pt%d" % si, ("mT", j)], ["pt%d" % si])
                            hv = (h // 4) * 128
                            mm(pO[0][:], vdd[:, j, hv:hv + 128], pt[si][:], j == 0, j == nk - 1, ["avd", "pt%d" % si], ["pO0"])
                            mm(pD[0][:], onesb[:], pt[si][:], j == 0, j == nk - 1, ["onesb", "pt%d" % si], ["pD0"])
                        T.op("dve", lambda e: e.reciprocal(out=rden[:], in_=pD[0][:]), ["pD0"], ["rden"])
                        T.op("dve", lambda e, q_=q_: e.tensor_tensor(out=yo[q_][:], in0=pO[0][:], in1=rden[:], op=ALU.mult),
                             ["pO0", "rden"], ["yo%d" % q_])
                        ld("sp", YT[1][h * 128:(h + 1) * 128, g * 512:(g + 1) * 512], yo[q_][:], ["yo%d" % q_], ["Y"])
                    if stop_after == "c0A2c":
                        T.barrier_all()
                        return
                T.barrier_all()
            chk_stop("A2")

            with ExitStack() as st:
                cw = sbt(st, "cw", [128, 3, 1024], F32)
                cb_ = sbt(st, "cb", [128, 1024], F32)
                for k3 in range(3):
                    ld("sp", cw[:, k3, :], conv_w[L][k3].partition_broadcast(128), [], ["cw"])
                ld("sp", cb_[:], conv_b[L].partition_broadcast(128), [], ["cb"])
                G = [[sbt(st, "G%d_%d" % (k3, i), [128, 1024], BF16) for i in range(2)] for k3 in range(3)]
                gbt = [sbt(st, "gbt%d" % i, [128, 1024], BF16) for i in range(2)]
                acc = sbt(st, "cacc", [128, 1024], F32)
                tm = sbt(st, "ctm", [128, 1024], F32)
                ycb = sbt(st, "ycb", [128, 1024], BF16)
                ycT = [sbt(st, "ycT%d" % i, [128, 8, 128], BF16) for i in range(2)]
                pbf = pst(st, "pbfc", [128, 1024], BF16)
                for i in range(16):
                    s_ = i % 2
                    rows = slice(i * 128, (i + 1) * 128)
                    for k3 in range(3):
                        gather(G[k3][s_][:], GCU[:, :], rw[:, k3, tcol(i):tcol(i) + 1], ["rw"], ["G%d_%d" % (k3, s_)])
                    ld("sp", gbt[s_][:], Zgb[rows, :], [], ["gbt%d" % s_])
                    T.op("dve", lambda e, s_=s_: e.tensor_tensor(out=acc[:], in0=G[0][s_][:], in1=cw[:, 0, :], op=ALU.mult),
                         ["G0_%d" % s_, "cw"], ["cacc"])
                    for k3 in (1, 2):
                        T.op("dve", lambda e, s_=s_, k3=k3: e.tensor_tensor(out=tm[:], in0=G[k3][s_][:], in1=cw[:, k3, :], op=ALU.mult),
                             ["G%d_%d" % (k3, s_), "cw"], ["ctm"])
                        T.op("dve", lambda e: e.tensor_tensor(out=acc[:], in0=acc[:], in1=tm[:], op=ALU.add), ["cacc", "ctm"], ["cacc"])
                    T.op("dve", lambda e: e.tensor_tensor(out=acc[:], in0=acc[:], in1=cb_[:], op=ALU.add), ["cacc", "cb"], ["cacc"])
                    T.op("dve", lambda e, s_=s_: e.tensor_tensor(out=ycb[:], in0=acc[:], in1=gbt[s_][:], op=ALU.mult),
                         ["cacc", "gbt%d" % s_], ["ycb"])
                    for c8 in range(8):
                        tp(pbf[:, c8 * 128:(c8 + 1) * 128], ycb[:, c8 * 128:(c8 + 1) * 128], identb[:], ["ycb", "identb"], ["pbfc"])
                    act(ycT[s_][:].rearrange("p a b -> p (a b)"), pbf[:], AF.Copy, ["pbfc"], ["ycT%d" % s_])
                    ld("sp", YT[2][:, rows].rearrange("(c p) t -> p c t", p=128), ycT[s_][:], ["ycT%d" % s_], ["Y"])
                T.barrier_all()
            chk_stop("C")

            with ExitStack() as st1:
                Y = [sbt(st1, "Y%d" % b, [128, 8, 2048], BF16) for b in range(3)]
                for b in range(3):
                    for c8 in range(0, 8, 2):
                        ld("sp", Y[b][:, c8:c8 + 2, :], YT[b].rearrange("(c p) t -> p c t", p=128)[:, c8:c8 + 2, :], [], ["Y%d" % b])
                wbr = [[sbt(st1, "wbr%d_%d" % (b, i), [128, 8, 512], BF16) for i in range(2)] for b in range(3)]
                gt = [sbt(st1, "gt%d" % i, [128, 3, 512], BF16) for i in range(2)]
                macc = sbt(st1, "macc", [128, 512], F32)
                mt2 = sbt(st1, "mt2", [128, 512], F32)
                mb = sbt(st1, "mb", [128, 512], BF16)
                mTs = [sbt(st1, "mTs%d" % i, [128, 4, 128], BF16) for i in range(2)]
                pB = [[pst(st1, "pB%d_%d" % (b, i), [128, 512]) for i in range(2)] for b in range(3)]
                pbf = pst(st1, "pbfb", [128, 1024], BF16)
                n = 0
                for cb4 in range(4):
                    s_ = cb4 % 2
                    cs = slice(cb4 * 512, (cb4 + 1) * 512)
                    for b in range(3):
                        ld("pool", wbr[b][s_][:], w_br[b][L].rearrange("(c p) n -> p c n", p=128)[:, :, cs], [], ["wbr%d_%d" % (b, s_)])
                    for i in range(16):
                        p_ = n % 2; n += 1
                        rows = slice(i * 128, (i + 1) * 128)
                        ld("sp", gt[p_][:], Zg[rows, :].rearrange("p (v n) -> p v n", v=3)[:, :, cs], [], ["gt%d" % p_])
                        for b in range(3):
                            for c8 in range(8):
                                mm(pB[b][p_][:], Y[b][:, c8, rows], wbr[b][s_][:, c8, :], c8 == 0, c8 == 7,
                                   ["Y%d" % b, "wbr%d_%d" % (b, s_)], ["pB%d_%d" % (b, p_)])
                        T.op("dve", lambda e, p_=p_: e.tensor_tensor(out=macc[:], in0=pB[0][p_][:], in1=gt[p_][:, 0, :], op=ALU.mult),
                             ["pB0_%d" % p_, "gt%d" % p_], ["macc"])
                        T.op("dve", lambda e, p_=p_: e.tensor_tensor(out=mt2[:], in0=pB[1][p_][:], in1=gt[p_][:, 1, :], op=ALU.mult),
                             ["pB1_%d" % p_, "gt%d" % p_], ["mt2"])
                        T.op("dve", lambda e: e.tensor_tensor(out=macc[:], in0=macc[:], in1=mt2[:], op=ALU.add), ["macc", "mt2"], ["macc"])
                        T.op("dve", lambda e, p_=p_: e.tensor_tensor(out=mt2[:], in0=pB[2][p_][:], in1=gt[p_][:, 2, :], op=ALU.mult),
                             ["pB2_%d" % p_, "gt%d" % p_], ["mt2"])
                        T.op("dve", lambda e: e.tensor_tensor(out=mb[:], in0=macc[:], in1=mt2[:], op=ALU.add), ["macc", "mt2"], ["mb"])
                        half = p_ * 512
                        for j in range(4):
                            tp(pbf[:, half + j * 128:half + (j + 1) * 128], mb[:, j * 128:(j + 1) * 128], identb[:], ["mb", "identb"], ["pbfb%d" % p_])
                        act(mTs[p_][:].rearrange("p a b -> p (a b)"), pbf[:, half:half + 512], AF.Copy, ["pbfb%d" % p_], ["mTs%d" % p_])
                        ld("sp", MT.rearrange("(c p) t -> p c t", p=128)[:, cb4 * 4:(cb4 + 1) * 4, rows], mTs[p_][:], ["mTs%d" % p_], ["MT"])
                T.barrier_all()
            chk_stop("B1")
            with ExitStack() as st:
                mTa = sbt(st, "mTa", [128, 16, 2048], BF16)
                Wo = sbt(st, "Wo", [128, 16, 2048], BF16)
                for c4 in range(0, 16, 2):
                    ld("sp", mTa[:, c4:c4 + 2, :], MT.rearrange("(c p) t -> p c t", p=128)[:, c4:c4 + 2, :], [], ["mTa"])
                for cb4 in range(4):
                    ld("pool", Wo[:, :, cb4 * 512:(cb4 + 1) * 512], w_o[L].rearrange("(c p) n -> p c n", p=128)[:, :, cb4 * 512:(cb4 + 1) * 512], [], ["Wo"])
                mo = [sbt(st, "mo%d" % i, [128, 512], F32) for i in range(3)]
                pW = [pst(st, "pW%d" % i, [128, 512]) for i in range(4)]
                n = 0
                for i in range(16):
                    rows = slice(i * 128, (i + 1) * 128)
                    for cb4 in range(4):
                        p_ = n % 4; m_ = n % 3; n += 1
                        for kc in range(16):
                            mm(pW[p_][:], mTa[:, kc, rows], Wo[:, kc, cb4 * 512:(cb4 + 1) * 512], kc == 0, kc == 15, ["mTa", "Wo"], ["pW%d" % p_])
                        act(mo[m_][:], pW[p_][:], AF.Copy, ["pW%d" % p_], ["mo%d" % m_])
                        ld("sp", MIX[rows, cb4 * 512:(cb4 + 1) * 512], mo[m_][:], ["mo%d" % m_], ["MIX"])
                T.barrier_all()
            chk_stop("B2a")
            with ExitStack() as st:
                gbc = sbt(st, "gbc", [128, D], F32)
                lng = sbt(st, "lng", [128, D], F32)
                lnb = sbt(st, "lnb", [128, D], F32)
                ld("sp", gbc[:], MODBC[L][:, 2 * D:3 * D], [], ["gbc"])
                ld("sp", lng[:], ln1_g[L].partition_broadcast(128), [], ["lng"])
                ld("sp", lnb[:], ln1_b[L].partition_broadcast(128), [], ["lnb"])
                xr = [sbt(st, "xr%d" % i, [128, D], F32) for i in range(2)]
                mx = [sbt(st, "mx%d" % i, [128, D], F32) for i in range(2)]
                xm = [sbt(st, "xm%d" % i, [128, D], F32) for i in range(2)]
                tt = sbt(st, "lntt", [128, D], F32)
                stats = sbt(st, "lnstats", [128, 4, 6], F32)
                mv = sbt(st, "lnmv", [128, 2], F32)
                rstd = sbt(st, "lnrstd", [128, 1], F32)
                h2T = [sbt(st, "h2T%d" % i, [128, 16, 128], BF16) for i in range(2)]
                pxs = [pst(st, "pxb%d" % i, [128, 512]) for i in range(2)]
                px = [(pxs[i], "pxb%d" % i) for i in range(2)]
                if moe:
                    h2f = sbt(st, "h2f", [128, 16, 128], F32)
                    wr = sbt(st, "wr", [128, 16, 8], F32)
                    ld("sp", wr[:], router_w[0].rearrange("(c p) e -> p c e", p=128), [], ["wr"])
                    lg = sbt(st, "lg", [128, 8], F32)
                    l8 = sbt(st, "l8", [128, 8], F32)
                    dlt = sbt(st, "dlt", [128, 1], F32)
                    g12 = sbt(st, "g12", [128, 2], F32)
                    e1 = sbt(st, "e1", [128, 8], F32)
                    e2 = sbt(st, "e2", [128, 8], F32)
                    gate = [sbt(st, "gate%d" % i, [128, 8], F32) for i in range(2)]
                    pR = pst(st, "pR", [128, 512])
                for i in range(16):
                    s_ = i % 2
                    rows = slice(i * 128, (i + 1) * 128)
                    gather(xr[s_][:], xin[:, :], rw[:, 0, tcol(i):tcol(i) + 1], ["rw"], ["xr%d" % s_])
                    ld("sp", mx[s_][:], MIX[rows, :], [], ["mx%d" % s_])
                    layernorm_tile((tt, stats, mv, rstd), [(mx[s_][:, j * 512:(j + 1) * 512], "mx%d" % s_) for j in range(4)],
                                   xr[s_], gbc, lng, lnb, xm[s_], ("xr%d" % s_, None, "xm%d" % s_))
                    ld("sp", XMID[rows, :], xm[s_][:], ["xm%d" % s_], ["XMID"])
                    build_hT(xm[s_], "xm%d" % s_, h2T[s_], "h2T%d" % s_, px, 3, 4, extra=((h2f, "h2f") if moe else None))
                    ld("sp", H2T.rearrange("(c p) t -> p c t", p=128)[:, :, rows], h2T[s_][:], ["h2T%d" % s_], ["H2T"])
                    if moe:
                        for kc in range(16):
                            mm(pR[:, 0:8], h2f[:, kc, :], wr[:, kc, :], kc == 0, kc == 15, [("h2f", kc), "wr"], ["pR"])
                        T.op("dve", lambda e: e.tensor_copy(out=lg[:], in_=pR[:, 0:8]), ["pR"], ["lg"])
                        T.op("dve", lambda e: e.max(out=l8[:], in_=lg[:]), ["lg"], ["l8"])
                        T.op("dve", lambda e: e.tensor_tensor(out=dlt[:], in0=l8[:, 0:1], in1=l8[:, 1:2], op=ALU.subtract), ["l8"], ["dlt"])
                        act(g12[:, 0:1], dlt[:], AF.Sigmoid, ["dlt"], ["g12a"])
                        act(g12[:, 1:2], dlt[:], AF.Sigmoid, ["dlt"], ["g12b"], scale=-1.0)
                        T.op("dve", lambda e: e.tensor_scalar(out=e1[:], in0=lg[:], scalar1=l8[:, 0:1], scalar2=g12[:, 0:1],
                                                              op0=ALU.is_equal, op1=ALU.mult), ["lg", "l8", "g12a"], ["e1"])
                        T.op("dve", lambda e: e.tensor_scalar(out=e2[:], in0=lg[:], scalar1=l8[:, 1:2], scalar2=g12[:, 1:2],
                                                              op0=ALU.is_equal, op1=ALU.mult), ["lg", "l8", "g12b"], ["e2"])
                        T.op("dve", lambda e, s_=s_: e.tensor_tensor(out=gate[s_][:], in0=e1[:], in1=e2[:], op=ALU.add), ["e1", "e2"], ["gate%d" % s_])
                        ld("sp", GATE[rows, :], gate[s_][:], ["gate%d" % s_], ["GATE"])
                    if i == 0:
                        try:
                            chk_stop("B2b0")
                        except StopBuild:
                            T.barrier_all()
                            raise
                T.barrier_all()
            chk_stop("B")

            with ExitStack() as st:
                dff = DFE if moe else DFF
                nfc = dff // 128
                nex = 8 if moe else 1
                FB = 256
                h2 = [sbt(st, "fh2_%d" % i, [128, 16, 512], BF16) for i in range(2)]
                hid = sbt(st, "hid", [128, nfc, 512], BF16)
                wg = [sbt(st, "wg%d" % i, [128, 16, FB], BF16) for i in range(2)]
                wu = [sbt(st, "wu%d" % i, [128, 16, FB], BF16) for i in range(2)]
                wd = [sbt(st, "wd%d" % i, [128, 4, 1024], BF16) for i in range(2)]
                sg = [sbt(st, "sg%d" % i, [128, 512], F32) for i in range(2)]
                facc = sbt(st, "facc", [128, 4, D], F32)
                gsb = sbt(st, "gsb", [128, 16, 8], F32)
                if moe:
                    ld("sp", gsb[:], GATE.rearrange("(i p) e -> p i e", p=128), [], ["gsb"])
                P = [pst(st, "pF%d" % i, [128, 512]) for i in range(8)]
                nw = 0
                nd = 0
                ng = 0
                for sc in range(4):
                    s_ = sc % 2
                    tsl = slice(sc * 512, (sc + 1) * 512)
                    ld("sp", h2[s_][:], H2T.rearrange("(c p) t -> p c t", p=128)[:, :, tsl], [], ["fh2_%d" % s_])
                    for ex in range(nex):
                        if moe:
                            Wg_, Wu_, Wd_ = moe_wg[0][ex], moe_wu[0][ex], moe_wd[0][ex]
                        else:
                            Wg_, Wu_, Wd_ = ffn_wg[0], ffn_wu[0], ffn_wd[0]
                        Wgv = Wg_.rearrange("(c p) n -> p c n", p=128)
                        Wuv = Wu_.rearrange("(c p) n -> p c n", p=128)
                        Wdv = Wd_.rearrange("(c p) n -> p c n", p=128)
                        for fb in range(dff // FB):
                            w_ = nw % 2; nw += 1
                            ld("pool", wg[w_][:], Wgv[:, :, fb * FB:(fb + 1) * FB], [], ["wg%d" % w_])
                            ld("pool", wu[w_][:], Wuv[:, :, fb * FB:(fb + 1) * FB], [], ["wu%d" % w_])
                            for f2 in range(FB // 128):
                                fc = fb * (FB // 128) + f2
                                p_ = ng % 2; ng += 1
                                bG, bU = P[p_ * 2], P[p_ * 2 + 1]
                                kG, kU = "pF%d" % (p_ * 2), "pF%d" % (p_ * 2 + 1)
                                for kc in range(16):
                                    mm(bG[:], wg[w_][:, kc, f2 * 128:(f2 + 1) * 128], h2[s_][:, kc, :], kc == 0, kc == 15,
                                       ["wg%d" % w_, "fh2_%d" % s_], [kG])
                                for kc in range(16):
                                    mm(bU[:], wu[w_][:, kc, f2 * 128:(f2 + 1) * 128], h2[s_][:, kc, :], kc == 0, kc == 15,
                                       ["wu%d" % w_, "fh2_%d" % s_], [kU])
                                act(sg[p_][:], bG[:], AF.Silu, [kG], ["sg%d" % p_])
                                T.op("dve", lambda e, p_=p_, fc=fc, bU=bU: e.tensor_tensor(out=hid[:, fc, :], in0=bU[:], in1=sg[p_][:], op=ALU.mult),
                                     [kU, "sg%d" % p_], ["hid"])
                        for chf in range(2):
                            for sl4 in range(nfc // 4):
                                d_ = nd % 2; nd += 1
                                ld("pool", wd[d_][:], Wdv[:, sl4 * 4:(sl4 + 1) * 4, chf * 1024:(chf + 1) * 1024], [], ["wd%d" % d_])
                                for f4 in range(4):
                                    fc = sl4 * 4 + f4
                                    for ti in range(4):
                                        for c2 in range(2):
                                            bk = ti * 2 + c2
                                            mm(P[bk][:], hid[:, fc, ti * 128:(ti + 1) * 128], wd[d_][:, f4, c2 * 512:(c2 + 1) * 512],
                                               fc == 0, fc == nfc - 1, ["hid", "wd%d" % d_], ["pF%d" % bk])
                            for ti in range(4):
                                gi_ = sc * 4 + ti
                                for c2 in range(2):
                                    bk = ti * 2 + c2
                                    cs = slice(chf * 1024 + c2 * 512, chf * 1024 + (c2 + 1) * 512)
                                    fk = ("facc", ti, chf, c2)
                                    if not moe:
                                        T.op("dve", lambda e, bk=bk, ti=ti, cs=cs: e.tensor_copy(out=facc[:, ti, cs], in_=P[bk][:]),
                                             ["pF%d" % bk], [fk])
                                    elif ex == 0:
                                        T.op("dve", lambda e, bk=bk, ti=ti, cs=cs, gi_=gi_, ex=ex: e.tensor_scalar(
                                            out=facc[:, ti, cs], in0=P[bk][:], scalar1=gsb[:, gi_, ex:ex + 1], scalar2=None, op0=ALU.mult),
                                            ["pF%d" % bk, "gsb"], [fk])
                                    else:
                                        T.op("dve", lambda e, bk=bk, ti=ti, cs=cs, gi_=gi_, ex=ex: e.scalar_tensor_tensor(
                                            out=facc[:, ti, cs], in0=P[bk][:], scalar=gsb[:, gi_, ex:ex + 1], in1=facc[:, ti, cs],
                                            op0=ALU.mult, op1=ALU.add), ["pF%d" % bk, "gsb", fk], [fk])
                    for ti in range(4):
                        i = sc * 4 + ti
                        ld("sp", FOUT[i * 128:(i + 1) * 128, :], facc[:, ti, :],
                           [("facc", ti, a, b) for a in range(2) for b in range(2)], ["FOUT"])
                T.barrier_all()
            with ExitStack() as st:
                gbc = sbt(st, "gbc", [128, D], F32)
                lng = sbt(st, "lng", [128, D], F32)
                lnb = sbt(st, "lnb", [128, D], F32)
                ld("sp", gbc[:], MODBC[L][:, 5 * D:6 * D], [], ["gbc"])
                ld("sp", lng[:], ln2_g[L].partition_broadcast(128), [], ["lng"])
                ld("sp", lnb[:], ln2_b[L].partition_broadcast(128), [], ["lnb"])
                xm = [sbt(st, "fxm%d" % i, [128, D], F32) for i in range(2)]
                ff = [sbt(st, "fff%d" % i, [128, D], F32) for i in range(2)]
                xo = [sbt(st, "fxo%d" % i, [128, D], F32) for i in range(2)]
                tt = sbt(st, "lntt", [128, D], F32)
                stats = sbt(st, "lnstats", [128, 4, 6], F32)
                mv = sbt(st, "lnmv", [128, 2], F32)
                rstd = sbt(st, "lnrstd", [128, 1], F32)
                for i in range(16):
                    x_ = i % 2
                    rows = slice(i * 128, (i + 1) * 128)
                    ld("sp", xm[x_][:], XMID[rows, :], [], ["fxm%d" % x_])
                    ld("sp", ff[x_][:], FOUT[rows, :], [], ["fff%d" % x_])
                    layernorm_tile((tt, stats, mv, rstd), [(ff[x_][:, j * 512:(j + 1) * 512], "fff%d" % x_) for j in range(4)],
                                   xm[x_], gbc, lng, lnb, xo[x_], ("fxm%d" % x_, None, "fxo%d" % x_))
                    orow = ch * 2048 + i * 128
                    ld("sp", xout[orow:orow + 128, :], xo[x_][:], ["fxo%d" % x_], ["XOUT"])
                T.barrier_all()


        try:
            layer(0, xb, 2, rw1, k_posrow, poscol, X1, False)
            layer(1, X1, 1, rw2, q2_posrow, q2pc, out, True)
        except StopBuild:
            pass
        T.barrier_all()
        build.ninst = T.ninst
        build.nops = T.nops
        build.lines = T.lines
    return nc


def host_tables(parity):
    ident = np.eye(128, dtype=np.float32)
    posrow = np.tile(np.arange(S, dtype=np.float32)[None, :], (128, 1))
    poscol = (np.arange(NT, dtype=np.float32)[None, :] * 128 + np.arange(128, dtype=np.float32)[:, None]).astype(np.float32)
    inv = np.concatenate([THETA ** (-np.arange(0, rd, 2, dtype=np.float32) / rd) for rd in (64, 32, 16)]).astype(np.float32)
    k_inv = np.tile(inv[None, :], (128, 1)).astype(np.float32)
    own_tiles = 2 * np.arange(16) + parity
    own_rows = (own_tiles[None, :] * 128 + np.arange(128)[:, None]).astype(np.int32)
    q2_posrow = np.tile(own_rows.T.reshape(-1).astype(np.float32)[None, :], (128, 1))
    q2_poscol = own_rows.astype(np.float32)
    r1 = (np.arange(NT)[None, :] * 128 + np.arange(128)[:, None]).astype(np.int32)
    rows1 = np.stack([r1, r1 + 1, r1 + 2]).astype(np.int32)
    rows2 = np.stack([own_rows, own_rows + 1, own_rows + 2]).astype(np.int32)
    return dict(k_ident=ident, k_posrow=posrow, k_poscol=poscol, k_inv=k_inv, q2_posrow=q2_posrow,
                q2_poscol=q2_poscol, rows1=rows1, rows2=rows2), own_rows


def make_in_map(inputs, core):
    b, p = core // 2, core % 2
    tabs, own_rows = host_tables(p)
    m = dict(tabs)
    m["xb"] = np.ascontiguousarray(inputs["x"][b])
    m["c_col"] = np.ascontiguousarray(inputs["c"][b].reshape(16, 128).T)
    m["pos_col"] = np.ascontiguousarray(inputs["positions"][b].reshape(NT, 128).T.astype(np.int32))
    for k in ("ada_w", "ada_b", "ln1_g", "ln1_b", "ln2_g", "ln2_b", "w_in", "mla_q_norm", "mla_kv_norm", "w_uq", "w_ukv",
              "conv_w", "conv_b", "w_branch_a", "w_branch_b", "w_branch_c", "w_o", "ffn_w_gate", "ffn_w_up", "ffn_w_down",
              "router_w", "moe_w_gate", "moe_w_up", "moe_w_down"):
        m[k] = inputs[k]
    return m, own_rows


def kernel(**inputs):
    inputs = {k: np.asarray(v) for k, v in inputs.items()}
    nc = build()
    maps = []
    owns = []
    for core in range(8):
        m, own = make_in_map(inputs, core)
        maps.append(m)
        owns.append(own)
    res = run_bass_kernel_spmd(nc, maps, core_ids=list(range(8)))
    outp = np.zeros((4, S, D), dtype=np.float32)
    for core in range(8):
        b = core // 2
        o = np.asarray(res.results[core]["out"]).reshape(16, 128, D)
        rows = owns[core].T
        for i in range(16):
            outp[b, rows[i]] = o[i]
    return outp
```

```python
import math
from contextlib import ExitStack
import numpy as np
import concourse.bass as bass
import concourse.mybir as mybir
from concourse.bass_utils import run_bass_kernel_spmd

F32 = mybir.dt.float32
BF16 = mybir.dt.bfloat16
I32 = mybir.dt.int32
AF = mybir.ActivationFunctionType
ALU = mybir.AluOpType

D = 2048
S = 4096
NT = S // 128
DEPTH = 2
ALPHA = (2 * DEPTH) ** 0.25
LN_EPS = 1e-5
RMS_EPS = 1e-6
THETA = 500000.0
DFF = 5632
DFE = 7168
NEG = -1.0e30
TWO_PI = 2.0 * math.pi
C_CQ, C_CKV, C_KR, C_Q, C_K, C_V, C_QI, C_KI, C_WI, C_GB, C_GC, C_U, C_G = (
    0, 512, 768, 832, 1856, 2112, 2368, 2880, 2944, 2952, 3976, 5000, 6024)
N_IN = 12168


class Trk:
    def __init__(self, nc, stack, n_dma_sems=56):
        self.nc = nc
        self.eng = {"pe": nc.tensor, "act": nc.scalar, "dve": nc.vector, "pool": nc.gpsimd, "sp": nc.sync}
        self.sem = {}
        self.cnt = {}
        for k in self.eng:
            self.sem[k] = stack.enter_context(nc.semaphore("s_" + k))
            self.cnt[k] = 0
        self.dsem = [stack.enter_context(nc.semaphore("d%d" % i)) for i in range(n_dma_sems)]
        self.dcnt = [0] * n_dma_sems
        self.dnext = 0
        self.known = {k: {} for k in self.eng}
        self.lastw = {}
        self.reads = {}
        self.ninst = 0
        self.nops = 0
        import os
        self.limit = int(os.environ.get("BISECT_N", "0")) or None
        self.reclines = bool(os.environ.get("RECLINES"))
        self.lines = []

    def _wait(self, e, tok):
        s, v, name, owner = tok
        if owner == "pe" and e == "pe":
            return
        kn = self.known[e]
        if kn.get(name, 0) >= v:
            return
        self.eng[e].wait_ge(s, v)
        kn[name] = v
        self.ninst += 1

    def _deps(self, e, reads, writes):
        for k in reads:
            t = self.lastw.get(k)
            if t is not None:
                self._wait(e, t)
        for k in writes:
            t = self.lastw.get(k)
            if t is not None:
                self._wait(e, t)
            for t in self.reads.get(k, ()):
                self._wait(e, t)

    def _commit(self, tok, reads, writes):
        for k in reads:
            self.reads.setdefault(k, []).append(tok)
        for k in writes:
            self.lastw[k] = tok
            self.reads[k] = []

    def _rec(self, e):
        import sys
        f = sys._getframe(2)
        self.lines.append((self.nops, e, f.f_lineno, f.f_back.f_lineno if f.f_back else 0))

    def op(self, e, fn, reads=(), writes=()):
        self.nops += 1
        if self.reclines:
            self._rec(e)
        if self.limit is not None and self.nops > self.limit:
            return None
        self._deps(e, reads, writes)
        ins = fn(self.eng[e])
        self.cnt[e] += 1
        ins.then_inc(self.sem[e], 1)
        tok = (self.sem[e], self.cnt[e], "s_" + e, e)
        self._commit(tok, reads, writes)
        self.ninst += 1
        return tok

    def dma(self, q, fn, reads=(), writes=()):
        self.nops += 1
        if self.reclines:
            self._rec("dma_" + q)
        if self.limit is not None and self.nops > self.limit:
            return None
        i = self.dnext
        self.dnext = (self.dnext + 1) % len(self.dsem)
        name = "d%d" % i
        if self.dcnt[i] > 0:
            self._wait(q, (self.dsem[i], self.dcnt[i], name, "dma"))
        self._deps(q, reads, writes)
        ins = fn(self.eng[q])
        self.dcnt[i] += 16
        ins.then_inc(self.dsem[i], 16)
        tok = (self.dsem[i], self.dcnt[i], name, "dma")
        self._commit(tok, reads, writes)
        self.ninst += 1
        return tok

    def barrier_all(self):
        toks = []
        for k in self.eng:
            if self.cnt[k] > 0:
                toks.append((self.sem[k], self.cnt[k], "s_" + k, k + "_x"))
        for i, s in enumerate(self.dsem):
            if self.dcnt[i] > 0:
                toks.append((s, self.dcnt[i], "d%d" % i, "dma"))
        for e in self.eng:
            for t in toks:
                if e == "pe" and t[2] == "s_pe":
                    continue
                self._wait(e, t)
        self.lastw.clear()
        self.reads.clear()


class StopBuild(Exception):
    pass


def build(dbg=(), stop_after=None):
    nc = bass.Bass("TRN2", target_bir_lowering=False)
    dbg = set(dbg)
    cur = {"L": 0, "ch": -1}

    def chk_stop(phase):
        if stop_after is None:
            return
        tgt = stop_after.split(":")
        if len(tgt) == 3 and int(tgt[0]) == cur["L"] and int(tgt[1]) == cur["ch"] and tgt[2] == phase:
            raise StopBuild()

    def din(name, shape, dt=F32):
        return nc.dram_tensor(name, list(shape), dt, kind="ExternalInput").ap()

    def dscr(name, shape, dt):
        kind = "ExternalOutput" if name in dbg else "Internal"
        return nc.dram_tensor(name, list(shape), dt, kind=kind).ap()

    xb = din("xb", [S, D])
    c_col = din("c_col", [128, 16])
    pos_col = din("pos_col", [128, NT], I32)
    ada_w = din("ada_w", [DEPTH, D, 6 * D])
    ada_b = din("ada_b", [DEPTH, 6 * D])
    ln1_g = din("ln1_g", [DEPTH, D]); ln1_b = din("ln1_b", [DEPTH, D])
    ln2_g = din("ln2_g", [DEPTH, D]); ln2_b = din("ln2_b", [DEPTH, D])
    w_in = din("w_in", [DEPTH, D, N_IN])
    q_norm = din("mla_q_norm", [DEPTH, 512]); kv_norm = din("mla_kv_norm", [DEPTH, 256])
    w_uq = din("w_uq", [DEPTH, 512, 1536]); w_ukv = din("w_ukv", [DEPTH, 256, 2048])
    conv_w = din("conv_w", [DEPTH, 3, 1024]); conv_b = din("conv_b", [DEPTH, 1024])
    w_br = [din("w_branch_a", [DEPTH, 1024, D]), din("w_branch_b", [DEPTH, 1024, D]), din("w_branch_c", [DEPTH, 1024, D])]
    w_o = din("w_o", [DEPTH, D, D])
    ffn_wg = din("ffn_w_gate", [1, D, DFF]); ffn_wu = din("ffn_w_up", [1, D, DFF]); ffn_wd = din("ffn_w_down", [1, DFF, D])
    router_w = din("router_w", [1, D, 8])
    lite = stop_after is not None and not stop_after.endswith(":F") or (stop_after is not None and stop_after.startswith("0:"))
    if lite:
        moe_wg = din("moe_w_gate", [1, 8, 128, 128]); moe_wu = din("moe_w_up", [1, 8, 128, 128]); moe_wd = din("moe_w_down", [1, 8, 128, 128])
    else:
        moe_wg = din("moe_w_gate", [1, 8, D, DFE]); moe_wu = din("moe_w_up", [1, 8, D, DFE]); moe_wd = din("moe_w_down", [1, 8, DFE, D])
    k_ident = din("k_ident", [128, 128])
    k_posrow = din("k_posrow", [128, S])
    k_poscol = din("k_poscol", [128, NT])
    k_inv = din("k_inv", [128, 56])
    q2_posrow = din("q2_posrow", [128, 2048])
    q2_poscol = din("q2_poscol", [128, 16])
    rows1 = din("rows1", [3, 128, NT], I32)
    rows2 = din("rows2", [3, 128, 16], I32)
    out = nc.dram_tensor("out", [2048, D], F32, kind="ExternalOutput").ap()

    ROPE = dscr("ROPE", [S, 112], F32)
    MODBC = dscr("MODBC", [DEPTH, 128, 6 * D], F32)
    X1 = dscr("X1", [S, D], F32)
    KnT = dscr("KnT", [8, 128, S], BF16)
    KrT = dscr("KrT", [64, S], BF16)
    KiT = dscr("KiT", [64, S], BF16)
    Vm = dscr("Vm", [S, 1024], BF16)
    KdT = dscr("KdT", [2, 128, S], BF16)
    Vd = dscr("Vd", [S, 256], BF16)
    GCU = dscr("GCU", [S + 2, 1024], BF16)
    Zcq = dscr("Zcq", [2048, 512], F32)
    Zq = dscr("Zq", [2048, 1024], F32)
    Zqi = dscr("Zqi", [2048, 512], F32)
    Zwi = dscr("Zwi", [2048, 8], F32)
    Zgb = dscr("Zgb", [2048, 1024], BF16)
    Zg = dscr("Zg", [2048, 6144], BF16)
    QnT = dscr("QnT", [8, 128, 2048], BF16)
    QrT = dscr("QrT", [8, 64, 2048], BF16)
    QdT = dscr("QdT", [8, 128, 2048], BF16)
    QiT = dscr("QiT", [8, 64, 2048], BF16)
    YT = [dscr("YaT", [1024, 2048], BF16), dscr("YbT", [1024, 2048], BF16), dscr("YcT", [1024, 2048], BF16)]
    XMID = dscr("XMID", [2048, D], F32)
    H2T = dscr("H2T", [D, 2048], BF16)
    GATE = dscr("GATE", [2048, 8], F32)
    MT = dscr("MT", [D, 2048], BF16)
    MIX = dscr("MIX", [2048, D], F32)
    FOUT = dscr("FOUT", [2048, D], F32)

    with ExitStack() as glob:
        T = Trk(nc, glob)

        uid = [0]

        def sbt(st, name, shape, dt):
            uid[0] += 1
            return st.enter_context(nc.sbuf_tensor("%s_u%d" % (name, uid[0]), list(shape), dt))

        def pst(st, name, shape, dt=F32):
            uid[0] += 1
            return st.enter_context(nc.psum_tensor("%s_u%d" % (name, uid[0]), list(shape), dt))

        def mm(o, l, r, start, stop, rd, wr):
            T.op("pe", lambda e: e.matmul(o, lhsT=l, rhs=r, start=start, stop=stop), rd, wr)

        def tp(o, i, ident, rd, wr):
            T.op("pe", lambda e: e.transpose(o, i, ident), rd, wr)

        def act(o, i, func, rd, wr, **kw):
            T.op("act", lambda e: e.activation(out=o, in_=i, func=func, **kw), rd, wr)

        def ld(q, o, i, rd, wr):
            wr = [k for k in wr if not (isinstance(k, str) and k[0].isupper() and k.isupper() or k in ("KnT", "KrT", "KiT", "Vm", "KdT", "Vd", "QnT", "QrT", "QdT", "QiT", "Y", "GCUpad"))]
            T.dma(q, lambda e: e.dma_start(out=o, in_=i), rd, wr)

        def gather(o, src, idx_ap, rd, wr):
            T.dma("pool", lambda e: e.indirect_dma_start(
                out=o, out_offset=None, in_=src,
                in_offset=bass.IndirectOffsetOnAxis(ap=idx_ap, axis=0)), rd, wr)

        identf = sbt(glob, "identf", [128, 128], F32)
        identb = sbt(glob, "identb", [128, 128], BF16)
        onesb = sbt(glob, "onesb", [128, 128], BF16)
        poscol = sbt(glob, "poscol", [128, NT], F32)
        q2pc = sbt(glob, "q2pc", [128, 16], F32)
        rw1 = sbt(glob, "rw1", [128, 3, NT], I32)
        rw2 = sbt(glob, "rw2", [128, 3, 16], I32)
        modT = sbt(glob, "modT", [128, DEPTH, 96], F32)
        ld("sp", identf[:], k_ident[:, :], [], ["identf"])
        ld("sp", poscol[:], k_poscol[:, :], [], ["poscol"])
        ld("sp", q2pc[:], q2_poscol[:, :], [], ["q2pc"])
        for j in range(3):
            ld("sp", rw1[:, j, :], rows1[j], [], ["rw1"])
            ld("sp", rw2[:, j, :], rows2[j], [], ["rw2"])
        T.op("dve", lambda e: e.tensor_copy(out=identb[:], in_=identf[:]), ["identf"], ["identb"])
        T.op("dve", lambda e: e.memset(onesb[:], 1.0), [], ["onesb"])

        with ExitStack() as st:
            posi = sbt(st, "posi", [128, NT], I32)
            posf = sbt(st, "posf", [128, NT], F32)
            inv = sbt(st, "inv", [128, 56], F32)
            ang = sbt(st, "ang", [128, NT, 56], F32)
            a2 = sbt(st, "a2", [128, NT, 56], F32)
            tab = sbt(st, "tab", [128, NT, 112], F32)
            ld("sp", posi[:], pos_col[:, :], [], ["posi"])
            ld("sp", inv[:], k_inv[:, :], [], ["inv"])
            T.op("dve", lambda e: e.tensor_copy(out=posf[:], in_=posi[:]), ["posi"], ["posf"])
            for t in range(NT):
                T.op("dve", lambda e, t=t: e.tensor_scalar(out=ang[:, t, :], in0=inv[:], scalar1=posf[:, t:t + 1],
                                                           scalar2=None, op0=ALU.mult), ["posf", "inv"], ["ang"])
            ki = sbt(st, "kint", [128, NT, 56], I32)
            kf = sbt(st, "kflt", [128, NT, 56], F32)
            C1 = 6.28125
            C2 = TWO_PI - C1

            def reduce_sin(shift, dst, kd):
                if shift != 0.0:
                    T.op("dve", lambda e: e.tensor_scalar(out=a2[:], in0=ang[:], scalar1=shift, scalar2=None, op0=ALU.add), ["ang", "tabs"], ["a2"])
                    src = a2
                else:
                    src = ang
                T.op("dve", lambda e: e.tensor_scalar(out=kf[:], in0=src[:], scalar1=1.0 / TWO_PI, scalar2=None, op0=ALU.mult), ["ang", "a2"], ["kflt"])
                T.op("dve", lambda e: e.tensor_copy(out=ki[:], in_=kf[:]), ["kflt"], ["kint"])
                T.op("dve", lambda e: e.tensor_copy(out=kf[:], in_=ki[:]), ["kint"], ["kflt"])
                T.op("dve", lambda e: e.scalar_tensor_tensor(out=a2[:], in0=kf[:], scalar=-C1, in1=src[:], op0=ALU.mult, op1=ALU.add), ["kflt", "ang", "a2"], ["a2"])
                T.op("dve", lambda e: e.scalar_tensor_tensor(out=a2[:], in0=kf[:], scalar=-C2, in1=a2[:], op0=ALU.mult, op1=ALU.add), ["kflt", "a2"], ["a2"])
                T.op("dve", lambda e: e.tensor_scalar(out=kf[:], in0=a2[:], scalar1=math.pi, scalar2=-TWO_PI, op0=ALU.is_gt, op1=ALU.mult), ["a2"], ["kflt"])
                T.op("dve", lambda e: e.tensor_tensor(out=a2[:], in0=a2[:], in1=kf[:], op=ALU.add), ["a2", "kflt"], ["a2"])
                T.op("dve", lambda e: e.tensor_scalar(out=kf[:], in0=a2[:], scalar1=-math.pi, scalar2=TWO_PI, op0=ALU.is_lt, op1=ALU.mult), ["a2"], ["kflt"])
                T.op("dve", lambda e: e.tensor_tensor(out=a2[:], in0=a2[:], in1=kf[:], op=ALU.add), ["a2", "kflt"], ["a2"])
                T.op("dve", lambda e: e.tensor_scalar(out=a2[:], in0=a2[:], scalar1=-3.14159, scalar2=3.14159, op0=ALU.max, op1=ALU.min), ["a2"], ["a2"])
                act(dst, a2[:], AF.Sin, ["a2"], [kd])

            reduce_sin(0.0, tab[:, :, 56:112], "tabs")
            reduce_sin(math.pi / 2, tab[:, :, 0:56], "tabc")
            ld("sp", ROPE.rearrange("(t p) f -> p t f", p=128), tab[:], ["tabs", "tabc"], ["ROPE"])
            T.barrier_all()

        def rope(src, dst, cos, sin, H, half, tmp, kin, kout, ktmp):
            x1 = src[:, :, 0:half]; x2 = src[:, :, half:2 * half]
            cb = cos.unsqueeze(1).to_broadcast([128, H, half]); sb_ = sin.unsqueeze(1).to_broadcast([128, H, half])
            t1 = tmp[:, 0, 0:H, 0:half]; t2 = tmp[:, 1, 0:H, 0:half]
            dv = lambda fn, r, w: T.op("dve", fn, r, w)
            dv(lambda e: e.tensor_tensor(out=t1, in0=x1, in1=cb, op=ALU.mult), kin, [ktmp + "1"])
            dv(lambda e: e.tensor_tensor(out=t2, in0=x2, in1=sb_, op=ALU.mult), kin, [ktmp + "2"])
            dv(lambda e: e.tensor_tensor(out=dst[:, :, 0:half], in0=t1, in1=t2, op=ALU.subtract), [ktmp + "1", ktmp + "2"], kout)
            dv(lambda e: e.tensor_tensor(out=t1, in0=x2, in1=cb, op=ALU.mult), kin, [ktmp + "1"])
            dv(lambda e: e.tensor_tensor(out=t2, in0=x1, in1=sb_, op=ALU.mult), kin, [ktmp + "2"])
            dv(lambda e: e.tensor_tensor(out=dst[:, :, half:2 * half], in0=t1, in1=t2, op=ALU.add), [ktmp + "1", ktmp + "2"], kout)
            Dh = src.shape[2]
            if Dh > 2 * half:
                dv(lambda e: e.tensor_copy(out=dst[:, :, 2 * half:Dh], in_=src[:, :, 2 * half:Dh]), kin, kout)

        def layernorm_tile(st_tiles, y_ps_list, xres, gbc, lng, lnb, dst, keys):
            tt, stats, mv, rstd = st_tiles
            kx, ky, kd = keys
            for j in range(4):
                sl = slice(j * 512, (j + 1) * 512)
                T.op("dve", lambda e, j=j, sl=sl: e.tensor_tensor(out=tt[:, sl], in0=y_ps_list[j][0], in1=gbc[:, sl], op=ALU.mult),
                     [y_ps_list[j][1], "gbc"], ["ln_tt%d" % j])
                T.op("dve", lambda e, sl=sl: e.scalar_tensor_tensor(out=tt[:, sl], in0=xres[:, sl], scalar=ALPHA, in1=tt[:, sl],
                                                                    op0=ALU.mult, op1=ALU.add), [kx, "ln_tt%d" % j], ["ln_tt%d" % j])
                T.op("dve", lambda e, j=j, sl=sl: e.bn_stats(out=stats[:, j, :], in_=tt[:, sl]), ["ln_tt%d" % j], ["ln_st%d" % j])
            T.op("dve", lambda e: e.bn_aggr(out=mv[:], in_=stats[:].rearrange("p a b -> p (a b)")), ["ln_st%d" % j for j in range(4)], ["ln_mv"])
            T.op("dve", lambda e: e.tensor_scalar(out=rstd[:], in0=mv[:, 1:2], scalar1=LN_EPS, scalar2=None, op0=ALU.add),
                 ["ln_mv"], ["ln_rstd"])
            act(rstd[:], rstd[:], AF.Sqrt, ["ln_rstd"], ["ln_rstd"])
            T.op("dve", lambda e: e.reciprocal(out=rstd[:], in_=rstd[:]), ["ln_rstd"], ["ln_rstd"])
            for j in range(4):
                sl = slice(j * 512, (j + 1) * 512)
                T.op("dve", lambda e, sl=sl: e.tensor_scalar(out=tt[:, sl], in0=tt[:, sl], scalar1=mv[:, 0:1], scalar2=rstd[:, 0:1],
                                                             op0=ALU.subtract, op1=ALU.mult), ["ln_mv", "ln_rstd", "ln_tt%d" % j], ["ln_tt%d" % j])
                T.op("pool", lambda e, sl=sl: e.tensor_tensor(out=tt[:, sl], in0=tt[:, sl], in1=lng[:, sl], op=ALU.mult),
                     ["ln_tt%d" % j, "lng"], ["ln_tt%d" % j])
                T.op("pool", lambda e, sl=sl: e.tensor_tensor(out=dst[:, sl], in0=tt[:, sl], in1=lnb[:, sl], op=ALU.add),
                     ["ln_tt%d" % j, "lnb"], [kd])

        def layer(L, xin, nchunk, rw, qposrow_dram, qpc, xout, moe):
            cur["L"] = L
            cur["ch"] = -1
            winL = w_in[L].rearrange("(c p) n -> p c n", p=128)
            with ExitStack() as st:
                ccol = sbt(st, "ccol", [128, 16], F32)
                cbc = sbt(st, "cbc", [128, 16, 128], F32)
                awb = [sbt(st, "awb%d" % i, [128, 16, 512], F32) for i in range(2)]
                abb = [sbt(st, "abb%d" % i, [128, 512], F32) for i in range(2)]
                mtmp = [sbt(st, "mtmp%d" % i, [128, 512], F32) for i in range(2)]
                pm = [pst(st, "pm%d" % i, [128, 512]) for i in range(2)]
                pt_ = [pst(st, "ptm%d" % i, [128, 512]) for i in range(2)]
                ld("sp", ccol[:], c_col[:, :], [], ["ccol"])
                for kc in range(16):
                    act(cbc[:, kc, :], ccol[:, kc:kc + 1].to_broadcast([128, 128]), AF.Silu, ["ccol"], ["cbc"])
                awv = ada_w[L].rearrange("(c p) n -> p c n", p=128)
                for b in range(24):
                    s_ = b % 2
                    ld("sp", awb[s_][:], awv[:, :, b * 512:(b + 1) * 512], [], ["awb%d" % s_])
                    ld("sp", abb[s_][:], ada_b[L][b * 512:(b + 1) * 512].partition_broadcast(128), [], ["abb%d" % s_])
                    for kc in range(16):
                        mm(pm[s_][:], cbc[:, kc, :], awb[s_][:, kc, :], kc == 0, kc == 15, ["cbc", "awb%d" % s_], ["pm%d" % s_])
                    T.op("dve", lambda e, s_=s_: e.tensor_tensor(out=mtmp[s_][:], in0=pm[s_][:], in1=abb[s_][:], op=ALU.add),
                         ["pm%d" % s_, "abb%d" % s_], ["mtmp%d" % s_])
                    ld("sp", MODBC[L][:, b * 512:(b + 1) * 512], mtmp[s_][:], ["mtmp%d" % s_], ["MODBC"])
                    for j in range(4):
                        tp(pt_[s_][:, j * 128:(j + 1) * 128], mtmp[s_][:, j * 128:(j + 1) * 128], identf[:], ["mtmp%d" % s_, "identf"], ["ptm%d" % s_])
                    col = b * 4
                    v = col // 16
                    addc = 1.0 if v in (1, 4) else 0.0
                    T.op("dve", lambda e, s_=s_, col=col, addc=addc: e.tensor_scalar(
                        out=modT[:, L, col:col + 4], in0=pt_[s_][:].rearrange("p (j c) -> p j c", c=128)[:, :, 0],
                        scalar1=addc, scalar2=None, op0=ALU.add), ["ptm%d" % s_], ["modT"])
                T.barrier_all()
            chk_stop("ada")

            def build_hT(xt_ap, kx, hT_dst, khT, px, vsh, vsc, extra=None):
                for q4 in range(4):
                    b = px[q4 % 2]
                    for j in range(4):
                        kc = q4 * 4 + j
                        tp(b[0][:, j * 128:(j + 1) * 128], xt_ap[:, kc * 128:(kc + 1) * 128], identf[:], [kx, "identf"], [b[1]])
                    for j in range(4):
                        kc = q4 * 4 + j
                        if extra is None:
                            act(hT_dst[:, kc, :], b[0][:, j * 128:(j + 1) * 128], AF.Identity, [b[1], "modT"], [khT],
                                scale=modT[:, L, vsc * 16 + kc:vsc * 16 + kc + 1], bias=modT[:, L, vsh * 16 + kc:vsh * 16 + kc + 1])
                        else:
                            ek = (extra[1], kc)
                            act(extra[0][:, kc, :], b[0][:, j * 128:(j + 1) * 128], AF.Identity, [b[1], "modT"], [ek],
                                scale=modT[:, L, vsc * 16 + kc:vsc * 16 + kc + 1], bias=modT[:, L, vsh * 16 + kc:vsh * 16 + kc + 1])
                            T.op("dve", lambda e, kc=kc: e.tensor_copy(out=hT_dst[:, kc, :], in_=extra[0][:, kc, :]), [ek], [khT])

            with ExitStack() as st:
                Wk = sbt(st, "Wk", [128, 16, 2944], BF16)
                wukv = sbt(st, "wukv", [128, 2, 2048], BF16)
                kvn = sbt(st, "kvn", [128, 256], F32)
                xt = [sbt(st, "xt%d" % i, [128, D], F32) for i in range(2)]
                hT = [sbt(st, "hT%d" % i, [128, 16, 128], BF16) for i in range(2)]
                rp = [sbt(st, "rp%d" % i, [128, 112], F32) for i in range(2)]
                Asb = sbt(st, "Asb", [128, 384], F32)
                Bsb = sbt(st, "Bsb", [128, 512], F32)
                gcs = sbt(st, "gcs", [128, 1024], F32)
                sq = sbt(st, "sq", [128, 256], F32)
                ssq = sbt(st, "ssq", [128, 1], F32)
                rstd = sbt(st, "rstdk", [128, 1], F32)
                ckvn = sbt(st, "ckvn", [128, 256], BF16)
                ckvT = sbt(st, "ckvT", [128, 2, 128], BF16)
                rtmp = sbt(st, "rtmp", [128, 2, 8, 32], F32)
                knT = [sbt(st, "knT%d" % i, [128, 8, 128], BF16) for i in range(2)]
                vt = [sbt(st, "vt%d" % i, [128, 1024], BF16) for i in range(2)]
                krki = sbt(st, "krki", [128, 2, 64], BF16)
                krkiT = [sbt(st, "krkiT%d" % i, [128, 128], BF16) for i in range(2)]
                kd = sbt(st, "kd", [128, 2, 128], BF16)
                kdT = [sbt(st, "kdT%d" % i, [128, 2, 128], BF16) for i in range(2)]
                vd = [sbt(st, "vd%d" % i, [128, 256], BF16) for i in range(2)]
                gcu = [sbt(st, "gcu%d" % i, [128, 1024], BF16) for i in range(2)]
                zpad = sbt(st, "zpad", [2, 1024], BF16)
                pxs = [pst(st, "pxk%d" % i, [128, 512]) for i in range(2)]
                pps = [pst(st, "ppk%d" % i, [128, 512]) for i in range(2)]
                pms = [pst(st, "pmk%d" % i, [128, 512]) for i in range(2)]
                pbf = pst(st, "pbfk", [128, 1024], BF16)
                px = [(pxs[i], "pxk%d" % i) for i in range(2)]
                segs = [(0, C_CKV, 320), (320, C_KI, 64), (384, C_K, 512), (896, C_GC, 2048)]
                for (lo, src, n) in segs:
                    for o in range(0, n, 512):
                        w_ = min(512, n - o)
                        ld("pool", Wk[:, :, lo + o:lo + o + w_], winL[:, :, src + o:src + o + w_], [], ["Wk"])
                ld("pool", wukv[:], w_ukv[L].rearrange("(c p) n -> p c n", p=128), [], ["wukv"])
                ld("sp", kvn[:], kv_norm[L].partition_broadcast(128), [], ["kvn"])
                T.op("dve", lambda e: e.memset(zpad[:], 0.0), [], ["zpad"])
                ld("sp", GCU[0:2, :], zpad[:], ["zpad"], ["GCUpad"])
                groups = [(0, 384), (384, 896), (896, 1408), (1408, 1920), (1920, 2432), (2432, 2944)]
                for t in range(NT):
                    s_ = t % 2
                    r0 = t * 128
                    kx = "xt%d" % s_
                    ld("sp", xt[s_][:], xin[r0:r0 + 128, :], [], [kx])
                    ld("sp", rp[s_][:], ROPE[r0:r0 + 128, :], [], ["rp%d" % s_])
                    khT = "hT%d" % s_
                    build_hT(xt[s_], kx, hT[s_], khT, px, 0, 1)
                    cosm, sinm = rp[s_][:, 0:32], rp[s_][:, 56:88]
                    cosd, sind = rp[s_][:, 32:48], rp[s_][:, 88:104]
                    cosi, sini = rp[s_][:, 48:56], rp[s_][:, 104:112]
                    krp = "rp%d" % s_
                    for gi, (lo, hi) in enumerate(groups):
                        pb = pps[gi % 2]; kp = "ppk%d" % (gi % 2)
                        n = hi - lo
                        for kc in range(16):
                            mm(pb[:, 0:n], hT[s_][:, kc, :], Wk[:, kc, lo:hi], kc == 0, kc == 15, [khT, "Wk"], [kp])
                        if gi == 0:
                            act(Asb[:], pb[:, 0:384], AF.Copy, [kp], ["Asb"])
                            act(sq[:], Asb[:, 0:256], AF.Square, ["Asb"], ["sq", "ssq"], accum_out=ssq[:])
                            T.op("dve", lambda e: e.tensor_scalar(out=rstd[:], in0=ssq[:], scalar1=1.0 / 256, scalar2=RMS_EPS,
                                                                  op0=ALU.mult, op1=ALU.add), ["ssq"], ["rstdk"])
                            act(rstd[:], rstd[:], AF.Sqrt, ["rstdk"], ["rstdk"])
                            T.op("dve", lambda e: e.reciprocal(out=rstd[:], in_=rstd[:]), ["rstdk"], ["rstdk"])
                            T.op("dve", lambda e: e.scalar_tensor_tensor(out=ckvn[:], in0=Asb[:, 0:256], scalar=rstd[:, 0:1], in1=kvn[:],
                                                                         op0=ALU.mult, op1=ALU.mult), ["Asb", "rstdk", "kvn"], ["ckvn"])
                            for c2 in range(2):
                                tp(pbf[:, c2 * 128:(c2 + 1) * 128], ckvn[:, c2 * 128:(c2 + 1) * 128], identb[:], ["ckvn", "identb"], ["pbfk"])
                            act(ckvT[:].rearrange("p a b -> p (a b)"), pbf[:, 0:256], AF.Copy, ["pbfk"], ["ckvT"])
                            for h in range(8):
                                pmb = pms[h // 4]; kpm = "pmk%d" % (h // 4)
                                for c2 in range(2):
                                    mm(pmb[:, (h % 4) * 128:(h % 4 + 1) * 128], wukv[:, c2, h * 256:h * 256 + 128], ckvT[:, c2, :],
                                       c2 == 0, c2 == 1, ["wukv", "ckvT"], [kpm])
                            for hh in range(2):
                                act(knT[s_][:, hh * 4:(hh + 1) * 4, :].rearrange("p a b -> p (a b)"), pms[hh][:], AF.Copy,
                                    ["pmk%d" % hh], ["knT%d" % s_])
                            ld("sp", KnT[:, :, r0:r0 + 128].rearrange("h d t -> d h t"), knT[s_][:], ["knT%d" % s_], ["KnT"])
                            wv = wukv[:].rearrange("p c (h d) -> p c h d", d=256)
                            for hh in range(2):
                                for c2 in range(2):
                                    mm(pms[hh][:], ckvT[:, c2, :], wv[:, c2, hh * 4:(hh + 1) * 4, 128:256], c2 == 0, c2 == 1,
                                       ["ckvT", "wukv"], ["pmk%d" % hh])
                                act(vt[s_][:, hh * 512:(hh + 1) * 512], pms[hh][:], AF.Copy, ["pmk%d" % hh], ["vt%d" % s_])
                            ld("sp", Vm[r0:r0 + 128, :], vt[s_][:], ["vt%d" % s_], ["Vm"])
                            rope(Asb[:, 256:320].rearrange("p (h d) -> p h d", h=1), krki[:, 0:1, :], cosm, sinm, 1, 32, rtmp,
                                 ["Asb", krp], ["krki"], "rtk")
                            rope(Asb[:, 320:384].rearrange("p (h d) -> p h d", h=1), krki[:, 1:2, :], cosi, sini, 1, 8, rtmp,
                                 ["Asb", krp], ["krki"], "rtk")
                            tp(pbf[:, 256:384], krki[:].rearrange("p a b -> p (a b)"), identb[:], ["krki", "identb"], ["pbfk2"])
                            act(krkiT[s_][:], pbf[:, 256:384], AF.Copy, ["pbfk2"], ["krkiT%d" % s_])
                            ld("sp", KrT[:, r0:r0 + 128], krkiT[s_][0:64, :], ["krkiT%d" % s_], ["KrT"])
                            ld("sp", KiT[:, r0:r0 + 128], krkiT[s_][64:128, :], ["krkiT%d" % s_], ["KiT"])
                        elif gi == 1:
                            act(Bsb[:], pb[:, 0:512], AF.Copy, [kp], ["Bsb"])
                            rope(Bsb[:, 0:256].rearrange("p (h d) -> p h d", h=2), kd[:], cosd, sind, 2, 16, rtmp,
                                 ["Bsb", krp], ["kd"], "rtk")
                            for g2 in range(2):
                                tp(pbf[:, 512 + g2 * 128:512 + (g2 + 1) * 128], kd[:, g2, :], identb[:], ["kd", "identb"], ["pbfk3"])
                            act(kdT[s_][:].rearrange("p a b -> p (a b)"), pbf[:, 512:768], AF.Copy, ["pbfk3"], ["kdT%d" % s_])
                            ld("sp", KdT[:, :, r0:r0 + 128].rearrange("g d t -> d g t"), kdT[s_][:], ["kdT%d" % s_], ["KdT"])
                            T.op("dve", lambda e, s_=s_: e.tensor_copy(out=vd[s_][:], in_=Bsb[:, 256:512]), ["Bsb"], ["vd%d" % s_])
                            ld("sp", Vd[r0:r0 + 128, :], vd[s_][:], ["vd%d" % s_], ["Vd"])
                        elif gi in (2, 3):
                            o = (gi - 2) * 512
                            act(gcs[:, o:o + 512], pb[:, 0:512], AF.Copy, [kp], ["gcs%d" % gi])
                        else:
                            o = (gi - 4) * 512
                            T.op("dve", lambda e, o=o, pb=pb, s_=s_: e.tensor_tensor(out=gcu[s_][:, o:o + 512], in0=pb[:, 0:512],
                                                                                in1=gcs[:, o:o + 512], op=ALU.mult),
                                 [kp, "gcs%d" % (gi - 2)], ["gcu%d" % s_])
                    ld("sp", GCU[2 + r0:2 + r0 + 128, :], gcu[s_][:], ["gcu%d" % s_], ["GCU"])
                T.barrier_all()
            chk_stop("K")

            for ch in range(nchunk):
                cur["ch"] = ch
                chunk(L, ch, winL, rw, qposrow_dram, qpc, xin, xout, moe, build_hT)
                chk_stop("F")
            cur["ch"] = -1

        def chunk(L, ch, winL, rw, qposrow_dram, qpc, xin, xout, moe, build_hT):
            tcol = lambda i: ch * 16 + i
            with ExitStack() as st:
                hTa = sbt(st, "hTa", [128, 16, 2048], BF16)
                xt = [sbt(st, "xq%d" % i, [128, D], F32) for i in range(2)]
                wb = [sbt(st, "wqb%d" % i, [128, 16, 512], BF16) for i in range(2)]
                zo = [sbt(st, "zo%d" % i, [128, 512], F32) for i in range(2)]
                zob = [sbt(st, "zob%d" % i, [128, 512], BF16) for i in range(2)]
                pxs = [pst(st, "pxq%d" % i, [128, 512]) for i in range(2)]
                pps = [pst(st, "ppq%d" % i, [128, 512]) for i in range(3)]
                px = [(pxs[i], "pxq%d" % i) for i in range(2)]
                for i in range(16):
                    s_ = i % 2
                    gather(xt[s_][:], xin[:, :], rw[:, 0, tcol(i):tcol(i) + 1], ["rw"], ["xq%d" % s_])
                    build_hT(xt[s_], "xq%d" % s_, hTa[:, :, i * 128:(i + 1) * 128], "hTa", px, 0, 1)
                blocks = [(C_CQ, 512, "cq", 0), (C_Q, 512, "q", 0), (C_Q + 512, 512, "q", 512), (C_QI, 512, "qi", 0),
                          (C_WI, 8, "wi", 0), (C_GB, 512, "gb", 0), (C_GB + 512, 512, "gb", 512)]
                blocks += [(C_G + j * 512, 512, "g", j * 512) for j in range(12)]
                dst = {"cq": Zcq, "q": Zq, "qi": Zqi, "wi": Zwi, "gb": Zgb, "g": Zg}
                cnt = 0
                for bi, (c0, n, kind, o) in enumerate(blocks):
                    s_ = bi % 2
                    ld("pool", wb[s_][:, :, 0:n], winL[:, :, c0:c0 + n], [], ["wqb%d" % s_])
                    for i in range(16):
                        pi = cnt % 3; zi = cnt % 2; cnt += 1
                        pb = pps[pi]; kp = "ppq%d" % pi
                        for kc in range(16):
                            mm(pb[:, 0:n], hTa[:, kc, i * 128:(i + 1) * 128], wb[s_][:, kc, 0:n], kc == 0, kc == 15,
                               ["hTa", "wqb%d" % s_], [kp])
                        rows = slice(i * 128, (i + 1) * 128)
                        if kind in ("g", "gb"):
                            act(zob[zi][:, 0:n], pb[:, 0:n], AF.Sigmoid if kind == "g" else AF.Copy, [kp], ["zob%d" % zi])
                            ld("sp", dst[kind][rows, o:o + n], zob[zi][:, 0:n], ["zob%d" % zi], [("Z", kind, i)])
                        else:
                            act(zo[zi][:, 0:n], pb[:, 0:n], AF.Copy, [kp], ["zo%d" % zi])
                            ld("sp", dst[kind][rows, o:o + n], zo[zi][:, 0:n], ["zo%d" % zi], [("Z", kind, i)])
                T.barrier_all()
            chk_stop("Q2")
            with ExitStack() as st:
                wuq = sbt(st, "wuq", [128, 4, 1536], BF16)
                qn = sbt(st, "qn", [128, 512], F32)
                ld("pool", wuq[:], w_uq[L].rearrange("(c p) n -> p c n", p=128), [], ["wuq"])
                ld("sp", qn[:], q_norm[L].partition_broadcast(128), [], ["qn"])
                cq = [sbt(st, "cq%d" % i, [128, 512], F32) for i in range(2)]
                qq = [sbt(st, "qq%d" % i, [128, 1024], F32) for i in range(2)]
                qi_ = [sbt(st, "qi%d" % i, [128, 512], F32) for i in range(2)]
                rp = [sbt(st, "rq%d" % i, [128, 112], F32) for i in range(2)]
                sq = sbt(st, "sqq", [128, 512], F32)
                ssq = sbt(st, "ssqq", [128, 1], F32)
                rstd = sbt(st, "rstdq", [128, 1], F32)
                cqn = sbt(st, "cqn", [128, 512], BF16)
                cqT = sbt(st, "cqT", [128, 4, 128], BF16)
                qrs = sbt(st, "qrs", [128, 512], F32)
                rtmp = sbt(st, "rtmpq", [128, 2, 8, 32], F32)
                qrb = sbt(st, "qrb", [128, 8, 64], BF16)
                qdb = sbt(st, "qdb", [128, 8, 128], BF16)
                qib = sbt(st, "qib", [128, 8, 64], BF16)
                qnT = [sbt(st, "qnT%d" % i, [128, 8, 128], BF16) for i in range(2)]
                qrT = [sbt(st, "qrT%d" % i, [128, 4, 128], BF16) for i in range(2)]
                qdT = [sbt(st, "qdT%d" % i, [128, 8, 128], BF16) for i in range(2)]
                qiT = [sbt(st, "qiT%d" % i, [128, 4, 128], BF16) for i in range(2)]
                pms = [pst(st, "pm3%d" % i, [128, 512]) for i in range(3)]
                pbfs = [pst(st, "pbf3%d" % i, [128, 1024], BF16) for i in range(2)]
                for i in range(16):
                    s_ = i % 2
                    rows = slice(i * 128, (i + 1) * 128)
                    ld("sp", cq[s_][:], Zcq[rows, :], [], ["cq%d" % s_])
                    ld("sp", qq[s_][:], Zq[rows, :], [], ["qq%d" % s_])
                    ld("sp", qi_[s_][:], Zqi[rows, :], [], ["qi%d" % s_])
                    gather(rp[s_][:], ROPE[:, :], rw[:, 0, tcol(i):tcol(i) + 1], ["rw"], ["rq%d" % s_])
                    krp = "rq%d" % s_
                    cosm, sinm = rp[s_][:, 0:32], rp[s_][:, 56:88]
                    cosd, sind = rp[s_][:, 32:48], rp[s_][:, 88:104]
                    cosi, sini = rp[s_][:, 48:56], rp[s_][:, 104:112]
                    act(sq[:], cq[s_][:], AF.Square, ["cq%d" % s_], ["sqq", "ssqq"], accum_out=ssq[:])
                    T.op("dve", lambda e: e.tensor_scalar(out=rstd[:], in0=ssq[:], scalar1=1.0 / 512, scalar2=RMS_EPS,
                                                          op0=ALU.mult, op1=ALU.add), ["ssqq"], ["rstdq"])
                    act(rstd[:], rstd[:], AF.Sqrt, ["rstdq"], ["rstdq"])
                    T.op("dve", lambda e: e.reciprocal(out=rstd[:], in_=rstd[:]), ["rstdq"], ["rstdq"])
                    T.op("dve", lambda e, s_=s_: e.scalar_tensor_tensor(out=cqn[:], in0=cq[s_][:], scalar=rstd[:, 0:1], in1=qn[:],
                                                                        op0=ALU.mult, op1=ALU.mult), ["cq%d" % s_, "rstdq", "qn"], ["cqn"])
                    for c4 in range(4):
                        tp(pbfs[0][:, c4 * 128:(c4 + 1) * 128], cqn[:, c4 * 128:(c4 + 1) * 128], identb[:], ["cqn", "identb"], ["pbf30a"])
                    act(cqT[:].rearrange("p a b -> p (a b)"), pbfs[0][:, 0:512], AF.Copy, ["pbf30a"], ["cqT"])
                    for h in range(8):
                        pmb = pms[h // 4]; kpm = "pm3%d" % (h // 4)
                        for c4 in range(4):
                            mm(pmb[:, (h % 4) * 128:(h % 4 + 1) * 128], wuq[:, c4, h * 192:h * 192 + 128], cqT[:, c4, :],
                               c4 == 0, c4 == 3, ["wuq", "cqT"], [kpm])
                    for hh in range(2):
                        act(qnT[s_][:, hh * 4:(hh + 1) * 4, :].rearrange("p a b -> p (a b)"), pms[hh][:], AF.Copy, ["pm3%d" % hh], ["qnT%d" % s_])
                    ld("sp", QnT[:, :, rows].rearrange("h d t -> d h t"), qnT[s_][:], ["qnT%d" % s_], ["QnT"])
                    wr_ = wuq[:].rearrange("p c (h d) -> p c h d", d=192)
                    for c4 in range(4):
                        mm(pms[2][:], cqT[:, c4, :], wr_[:, c4, :, 128:192], c4 == 0, c4 == 3, ["cqT", "wuq"], ["pm32"])
                    act(qrs[:], pms[2][:], AF.Copy, ["pm32"], ["qrs"])
                    rope(qrs[:].rearrange("p (h d) -> p h d", h=8), qrb[:], cosm, sinm, 8, 32, rtmp, ["qrs", krp], ["qrb"], "rtq")
                    for j in range(4):
                        tp(pbfs[0][:, 512 + j * 128:512 + (j + 1) * 128], qrb[:, 2 * j:2 * j + 2, :].rearrange("p a b -> p (a b)"), identb[:],
                           ["qrb", "identb"], ["pbf30b"])
                    act(qrT[s_][:].rearrange("p a b -> p (a b)"), pbfs[0][:, 512:1024], AF.Copy, ["pbf30b"], ["qrT%d" % s_])
                    for j in range(4):
                        ld("sp", QrT[2 * j:2 * j + 2, :, rows].rearrange("h d t -> (h d) t"), qrT[s_][:, j, :], ["qrT%d" % s_], ["QrT"])
                    rope(qq[s_][:].rearrange("p (h d) -> p h d", h=8), qdb[:], cosd, sind, 8, 16, rtmp, ["qq%d" % s_, krp], ["qdb"], "rtq")
                    for h in range(8):
                        tp(pbfs[1][:, h * 128:(h + 1) * 128], qdb[:, h, :], identb[:], ["qdb", "identb"], ["pbf31"])
                    act(qdT[s_][:].rearrange("p a b -> p (a b)"), pbfs[1][:], AF.Copy, ["pbf31"], ["qdT%d" % s_])
                    ld("sp", QdT[:, :, rows].rearrange("h d t -> d h t"), qdT[s_][:], ["qdT%d" % s_], ["QdT"])
                    rope(qi_[s_][:].rearrange("p (h d) -> p h d", h=8), qib[:], cosi, sini, 8, 8, rtmp, ["qi%d" % s_, krp], ["qib"], "rtq")
                    for j in range(4):
                        tp(pbfs[0][:, j * 128:(j + 1) * 128], qib[:, 2 * j:2 * j + 2, :].rearrange("p a b -> p (a b)"), identb[:],
                           ["qib", "identb"], ["pbf30a"])
                    act(qiT[s_][:].rearrange("p a b -> p (a b)"), pbfs[0][:, 0:512], AF.Copy, ["pbf30a"], ["qiT%d" % s_])
                    for j in range(4):
                        ld("sp", QiT[2 * j:2 * j + 2, :, rows].rearrange("h d t -> (h d) t"), qiT[s_][:, j, :], ["qiT%d" % s_], ["QiT"])
                T.barrier_all()
            chk_stop("Q3")

            if L == 0:
                kmax = [4 * (4 * ch + g) + 4 for g in range(4)]
                kfull = [4 * (4 * ch + g) for g in range(4)]
            else:
                kmax = [8 * g + 8 for g in range(4)]
                kfull = [8 * g for g in range(4)]

            def attn_core(st, heads, load_head, s_mm, vslice, mask_fn, scale, Ydst):
                pt = [sbt(st, "pt%d" % i, [128, 512], BF16) for i in range(3)]
                rden = sbt(st, "rden", [128, 512], F32)
                yo = [sbt(st, "yo%d" % i, [128, 512], BF16) for i in range(2)]
                pS = [pst(st, "pS%d" % i, [128, 512]) for i in range(3)]
                pO = [pst(st, "pO%d" % i, [128, 512]) for i in range(2)]
                pD = [pst(st, "pD%d" % i, [128, 512]) for i in range(2)]
                it = 0
                gi = 0
                for h in heads:
                    load_head(h)
                    for g in range(4):
                        o_ = gi % 2; gi += 1
                        nk = kmax[g]
                        for j in range(nk):
                            si = it % 3; it += 1
                            s_mm(h, g, j, pS[si], "pS%d" % si)
                            act(pt[si][:], pS[si][:], AF.Exp, ["pS%d" % si], ["pt%d" % si], scale=scale)
                            mk = mask_fn(g, j)
                            if mk is not None:
                                T.op("pool", lambda e, si=si, mk=mk: e.tensor_tensor(out=pt[si][:], in0=pt[si][:], in1=mk[0], op=ALU.mult),
                                     ["pt%d" % si, mk[1]], ["pt%d" % si])
                            va, kv = vslice(h, j)
                            mm(pO[o_][:], va, pt[si][:], j == 0, j == nk - 1, [kv, "pt%d" % si], ["pO%d" % o_])
                            mm(pD[o_][:], onesb[:], pt[si][:], j == 0, j == nk - 1, ["onesb", "pt%d" % si], ["pD%d" % o_])
                        T.op("dve", lambda e, o_=o_: e.reciprocal(out=rden[:], in_=pD[o_][:]), ["pD%d" % o_], ["rden"])
                        T.op("dve", lambda e, o_=o_: e.tensor_tensor(out=yo[o_][:], in0=pO[o_][:], in1=rden[:], op=ALU.mult),
                             ["pO%d" % o_, "rden"], ["yo%d" % o_])
                        ld("sp", Ydst[h * 128:(h + 1) * 128, g * 512:(g + 1) * 512], yo[o_][:], ["yo%d" % o_], ["Y"])

            with ExitStack() as st:
                qpr = sbt(st, "qpr", [128, 2048], F32)
                cm = sbt(st, "cm", [128, 4, 8, 512], BF16)
                krT = sbt(st, "krT", [64, S], BF16)
                knT = [sbt(st, "aknT%d" % i, [128, S], BF16) for i in range(2)]
                vv = [sbt(st, "avv%d" % i, [128, NT, 128], BF16) for i in range(2)]
                qnT = [sbt(st, "aqn%d" % i, [128, 2048], BF16) for i in range(2)]
                qrT = [sbt(st, "aqr%d" % i, [64, 2048], BF16) for i in range(2)]
                ld("sp", qpr[:], qposrow_dram[:, ch * 2048:(ch + 1) * 2048], [], ["qpr"])
                ld("sp", krT[:], KrT[:, :], [], ["krT"])
                nband = kmax[0] - kfull[0]
                for g in range(4):
                    for jj in range(nband):
                        j = kfull[g] + jj
                        T.op("dve", lambda e, g=g, jj=jj, j=j: e.tensor_scalar(out=cm[:, g, jj, :], in0=qpr[:, g * 512:(g + 1) * 512],
                                                                              scalar1=poscol[:, j:j + 1], scalar2=None, op0=ALU.is_ge),
                             ["qpr", "poscol"], ["cm"])
                hs = {}

                def load_head(h):
                    s_ = h % 2
                    hs["s"] = s_
                    ld("sp", knT[s_][:], KnT[h], [], ["aknT%d" % s_])
                    ld("sp", vv[s_][:], Vm[:, h * 128:(h + 1) * 128].rearrange("(j p) d -> p j d", p=128), [], ["avv%d" % s_])
                    ld("sp", qnT[s_][:], QnT[h], [], ["aqn%d" % s_])
                    ld("sp", qrT[s_][:], QrT[h], [], ["aqr%d" % s_])

                def s_mm(h, g, j, ps, kps):
                    s_ = h % 2
                    mm(ps[:], knT[s_][:, j * 128:(j + 1) * 128], qnT[s_][:, g * 512:(g + 1) * 512], True, False,
                       ["aknT%d" % s_, "aqn%d" % s_], [kps])
                    mm(ps[:], krT[:, j * 128:(j + 1) * 128], qrT[s_][:, g * 512:(g + 1) * 512], False, True,
                       ["krT", "aqr%d" % s_], [kps])

                def vslice(h, j):
                    return vv[h % 2][:, j, :], "avv%d" % (h % 2)

                def mask_fn(g, j):
                    if j < kfull[g]:
                        return None
                    return (cm[:, g, j - kfull[g], :], "cm")

                attn_core(st, list(range(8)), load_head, s_mm, vslice, mask_fn, 192.0 ** -0.5, YT[0])
                T.barrier_all()
            chk_stop("A1")

            with ExitStack() as st:
                prow = sbt(st, "prow", [128, S], F32)
                kiT = sbt(st, "kiT", [64, S], BF16)
                qiT = sbt(st, "aqi", [64, 8, 2048], BF16)
                wi = sbt(st, "wi", [128, 16, 8], F32)
                kdT = sbt(st, "akd", [128, 2, S], BF16)
                vdd = sbt(st, "avd", [128, NT, 256], BF16)
                SC = sbt(st, "SC", [128, S], F32)
                WK = sbt(st, "WK", [128, S], F32)
                Rr = [sbt(st, "Rr%d" % i, [128, 512], F32) for i in range(2)]
                bias = sbt(st, "cbias", [128, 1024], F32)
                m8 = sbt(st, "m8", [128, 8], F32)
                thr = sbt(st, "thr", [128, 1], F32)
                Mk = sbt(st, "Mk", [128, S], BF16)
                mT = sbt(st, "mT", [128, 32, 512], BF16)
                qd = [sbt(st, "aqd%d" % i, [128, 512], BF16) for i in range(2)]
                pI = [pst(st, "pI%d" % i, [128, 512]) for i in range(1)]
                ld("sp", prow[:], k_posrow[:, :], [], ["prow"])
                ld("sp", kiT[:], KiT[:, :], [], ["kiT"])
                ld("sp", qiT[:], QiT.rearrange("h d t -> d h t"), [], ["aqi"])
                ld("sp", wi[:], Zwi.rearrange("(i p) h -> p i h", p=128), [], ["wi"])
                ld("sp", kdT[:], KdT.rearrange("g d t -> d g t"), [], ["akd"])
                ld("sp", vdd[:], Vd.rearrange("(j p) d -> p j d", p=128), [], ["avd"])
                pt = [sbt(st, "pt%d" % i, [128, 512], BF16) for i in range(3)]
                rden = sbt(st, "rden", [128, 512], F32)
                yo = [sbt(st, "yo%d" % i, [128, 512], BF16) for i in range(2)]
                pS = [pst(st, "pS%d" % i, [128, 512]) for i in range(3)]
                pO = [pst(st, "pO%d" % i, [128, 512]) for i in range(1)]
                pD = [pst(st, "pD%d" % i, [128, 512]) for i in range(1)]
                pbf = pst(st, "pbfa", [128, 1024], BF16)
                it = 0
                for g in range(4):
                    nk = kmax[g]
                    E = nk * 128
                    band0 = kfull[g] * 128
                    for qi4 in range(4):
                        i = g * 4 + qi4
                        for kb in range(E // 512):
                            cs = slice(kb * 512, (kb + 1) * 512)
                            for hh in range(8):
                                r_ = hh % 2
                                mm(pI[0][:], qiT[:, hh, i * 128:(i + 1) * 128], kiT[:, cs], True, True, ["aqi", "kiT"], ["pI0"])
                                act(Rr[r_][:], pI[0][:], AF.Relu, ["pI0"], ["Rr%d" % r_])
                                if hh == 0:
                                    T.op("dve", lambda e, r_=r_, cs=cs, i=i, hh=hh: e.tensor_scalar(
                                        out=SC[:, cs], in0=Rr[r_][:], scalar1=wi[:, i, hh:hh + 1], scalar2=None, op0=ALU.mult),
                                        ["Rr%d" % r_, "wi"], ["SC"])
                                else:
                                    T.op("dve", lambda e, r_=r_, cs=cs, i=i, hh=hh: e.scalar_tensor_tensor(
                                        out=SC[:, cs], in0=Rr[r_][:], scalar=wi[:, i, hh:hh + 1], in1=SC[:, cs],
                                        op0=ALU.mult, op1=ALU.add), ["Rr%d" % r_, "wi", "SC"], ["SC"])
                        if stop_after == "c0A2a":
                            T.barrier_all()
                            return
                        bw = E - band0
                        T.op("dve", lambda e, i=i, band0=band0, bw=bw: e.tensor_scalar(
                            out=bias[:, 0:bw], in0=prow[:, band0:E], scalar1=qpc[:, tcol(i):tcol(i) + 1], scalar2=NEG,
                            op0=ALU.is_gt, op1=ALU.mult), ["prow"], ["cbias"])
                        T.op("dve", lambda e, band0=band0, bw=bw, E=E: e.tensor_tensor(out=SC[:, band0:E], in0=SC[:, band0:E], in1=bias[:, 0:bw],
                                                                                  op=ALU.add), ["SC", "cbias"], ["SC"])
                        src = SC
                        for r in range(32):
                            T.op("dve", lambda e, src=src, E=E: e.max(out=m8[:], in_=src[:, 0:E]), ["SC", "WK"], ["m8"])
                            if r < 31:
                                T.op("dve", lambda e, src=src, E=E: e.match_replace(out=WK[:, 0:E], in_to_replace=m8[:], in_values=src[:, 0:E],
                                                                                imm_value=NEG), ["m8", "SC", "WK"], ["WK"])
                            src = WK
                        T.op("dve", lambda e: e.tensor_scalar(out=thr[:], in0=m8[:, 7:8], scalar1=-1.0e29, scalar2=None, op0=ALU.max),
                             ["m8"], ["thr"])
                        T.op("dve", lambda e, E=E: e.tensor_scalar(out=Mk[:, 0:E], in0=SC[:, 0:E], scalar1=thr[:, 0:1], scalar2=None,
                                                                   op0=ALU.is_ge), ["SC", "thr"], ["Mk"])
                        for j4 in range(nk // 4):
                            half = 0
                            for jj in range(4):
                                j = j4 * 4 + jj
                                tp(pbf[:, half + jj * 128:half + (jj + 1) * 128], Mk[:, j * 128:(j + 1) * 128], identb[:],
                                   ["Mk", "identb"], ["pbfa0"])
                            for jj in range(4):
                                act(mT[:, j4 * 4 + jj, qi4 * 128:(qi4 + 1) * 128],
                                    pbf[:, half + jj * 128:half + (jj + 1) * 128], AF.Copy, ["pbfa0"], [("mT", j4 * 4 + jj)])
                    if stop_after == "c0A2b" or (stop_after == "c0A2d" and g == 1):
                        T.barrier_all()
                        return
                    for h in range(8):
                        q_ = h % 2
                        ld("sp", qd[q_][:], QdT[h][:, g * 512:(g + 1) * 512], [], ["aqd%d" % q_])
                        for j in range(nk):
                            si = it % 3; it += 1
                            mm(pS[si][:], kdT[:, h // 4, j * 128:(j + 1) * 128], qd[q_][:], True, True, ["akd", "aqd%d" % q_], ["pS%d" % si])
                            act(pt[si][:], pS[si][:], AF.Exp, ["pS%d" % si], ["pt%d" % si], scale=128.0 ** -0.5)
                            T.op("pool", lambda e, si=si, j=j: e.tensor_tensor(out=pt[si][:], in0=pt[si][:], in1=mT[:, j, :], op=ALU.mult),
                                 ["pt%d" % si, ("mT", j)], ["pt%d" % si])
                            hv = (h // 4) * 128
                            mm(pO[0][:], vdd[:, j, hv:hv + 128], pt[si][:], j == 0, j == nk - 1, ["avd", "pt%d" % si], ["pO0"])
                            mm(pD[0][:], onesb[:], pt[si][:], j == 0, j == nk - 1, ["onesb", "pt%d" % si], ["pD0"])
                        T.op("dve", lambda e: e.reciprocal(out=rden[:], in_=pD[0][:]), ["pD0"], ["rden"])
                        T.op("dve", lambda e, q_=q_: e.tensor_tensor(out=yo[q_][:], in0=pO[0][:], in1=rden[:], op=ALU.mult),
                             ["pO0", "rden"], ["yo%d" % q_])
                        ld("sp", YT[1][h * 128:(h + 1) * 128, g * 512:(g + 1) * 512], yo[q_][:], ["yo%d" % q_], ["Y"])
                    if stop_after == "c0A2c":
                        T.barrier_all()
                        return
                T.barrier_all()
            chk_stop("A2")

            with ExitStack() as st:
                cw = sbt(st, "cw", [128, 3, 1024], F32)
                cb_ = sbt(st, "cb", [128, 1024], F32)
                for k3 in range(3):
                    ld("sp", cw[:, k3, :], conv_w[L][k3].partition_broadcast(128), [], ["cw"])
                ld("sp", cb_[:], conv_b[L].partition_broadcast(128), [], ["cb"])
                G = [[sbt(st, "G%d_%d" % (k3, i), [128, 1024], BF16) for i in range(2)] for k3 in range(3)]
                gbt = [sbt(st, "gbt%d" % i, [128, 1024], BF16) for i in range(2)]
                acc = sbt(st, "cacc", [128, 1024], F32)
                tm = sbt(st, "ctm", [128, 1024], F32)
                ycb = sbt(st, "ycb", [128, 1024], BF16)
                ycT = [sbt(st, "ycT%d" % i, [128, 8, 128], BF16) for i in range(2)]
                pbf = pst(st, "pbfc", [128, 1024], BF16)
                for i in range(16):
                    s_ = i % 2
                    rows = slice(i * 128, (i + 1) * 128)
                    for k3 in range(3):
                        gather(G[k3][s_][:], GCU[:, :], rw[:, k3, tcol(i):tcol(i) + 1], ["rw"], ["G%d_%d" % (k3, s_)])
                    ld("sp", gbt[s_][:], Zgb[rows, :], [], ["gbt%d" % s_])
                    T.op("dve", lambda e, s_=s_: e.tensor_tensor(out=acc[:], in0=G[0][s_][:], in1=cw[:, 0, :], op=ALU.mult),
                         ["G0_%d" % s_, "cw"], ["cacc"])
                    for k3 in (1, 2):
                        T.op("dve", lambda e, s_=s_, k3=k3: e.tensor_tensor(out=tm[:], in0=G[k3][s_][:], in1=cw[:, k3, :], op=ALU.mult),
                             ["G%d_%d" % (k3, s_), "cw"], ["ctm"])
                        T.op("dve", lambda e: e.tensor_tensor(out=acc[:], in0=acc[:], in1=tm[:], op=ALU.add), ["cacc", "ctm"], ["cacc"])
                    T.op("dve", lambda e: e.tensor_tensor(out=acc[:], in0=acc[:], in1=cb_[:], op=ALU.add), ["cacc", "cb"], ["cacc"])
                    T.op("dve", lambda e, s_=s_: e.tensor_tensor(out=ycb[:], in0=acc[:], in1=gbt[s_][:], op=ALU.mult),
                         ["cacc", "gbt%d" % s_], ["ycb"])
                    for c8 in range(8):
                        tp(pbf[:, c8 * 128:(c8 + 1) * 128], ycb[:, c8 * 128:(c8 + 1) * 128], identb[:], ["ycb", "identb"], ["pbfc"])
                    act(ycT[s_][:].rearrange("p a b -> p (a b)"), pbf[:], AF.Copy, ["pbfc"], ["ycT%d" % s_])
                    ld("sp", YT[2][:, rows].rearrange("(c p) t -> p c t", p=128), ycT[s_][:], ["ycT%d" % s_], ["Y"])
                T.barrier_all()
            chk_stop("C")

            with ExitStack() as st1:
                Y = [sbt(st1, "Y%d" % b, [128, 8, 2048], BF16) for b in range(3)]
                for b in range(3):
                    for c8 in range(0, 8, 2):
                        ld("sp", Y[b][:, c8:c8 + 2, :], YT[b].rearrange("(c p) t -> p c t", p=128)[:, c8:c8 + 2, :], [], ["Y%d" % b])
                wbr = [[sbt(st1, "wbr%d_%d" % (b, i), [128, 8, 512], BF16) for i in range(2)] for b in range(3)]
                gt = [sbt(st1, "gt%d" % i, [128, 3, 512], BF16) for i in range(2)]
                macc = sbt(st1, "macc", [128, 512], F32)
                mt2 = sbt(st1, "mt2", [128, 512], F32)
                mb = sbt(st1, "mb", [128, 512], BF16)
                mTs = [sbt(st1, "mTs%d" % i, [128, 4, 128], BF16) for i in range(2)]
                pB = [[pst(st1, "pB%d_%d" % (b, i), [128, 512]) for i in range(2)] for b in range(3)]
                pbf = pst(st1, "pbfb", [128, 1024], BF16)
                n = 0
                for cb4 in range(4):
                    s_ = cb4 % 2
                    cs = slice(cb4 * 512, (cb4 + 1) * 512)
                    for b in range(3):
                        ld("pool", wbr[b][s_][:], w_br[b][L].rearrange("(c p) n -> p c n", p=128)[:, :, cs], [], ["wbr%d_%d" % (b, s_)])
                    for i in range(16):
                        p_ = n % 2; n += 1
                        rows = slice(i * 128, (i + 1) * 128)
                        ld("sp", gt[p_][:], Zg[rows, :].rearrange("p (v n) -> p v n", v=3)[:, :, cs], [], ["gt%d" % p_])
                        for b in range(3):
                            for c8 in range(8):
                                mm(pB[b][p_][:], Y[b][:, c8, rows], wbr[b][s_][:, c8, :], c8 == 0, c8 == 7,
                                   ["Y%d" % b, "wbr%d_%d" % (b, s_)], ["pB%d_%d" % (b, p_)])
                        T.op("dve", lambda e, p_=p_: e.tensor_tensor(out=macc[:], in0=pB[0][p_][:], in1=gt[p_][:, 0, :], op=ALU.mult),
                             ["pB0_%d" % p_, "gt%d" % p_], ["macc"])
                        T.op("dve", lambda e, p_=p_: e.tensor_tensor(out=mt2[:], in0=pB[1][p_][:], in1=gt[p_][:, 1, :], op=ALU.mult),
                             ["pB1_%d" % p_, "gt%d" % p_], ["mt2"])
                        T.op("dve", lambda e: e.tensor_tensor(out=macc[:], in0=macc[:], in1=mt2[:], op=ALU.add), ["macc", "mt2"], ["macc"])
                        T.op("dve", lambda e, p_=p_: e.tensor_tensor(out=mt2[:], in0=pB[2][p_][:], in1=gt[p_][:, 2, :], op=ALU.mult),
                             ["pB2_%d" % p_, "gt%d" % p_], ["mt2"])
                        T.op("dve", lambda e: e.tensor_tensor(out=mb[:], in0=macc[:], in1=mt2[:], op=ALU.add), ["macc", "mt2"], ["mb"])
                        half = p_ * 512
                        for j in range(4):
                            tp(pbf[:, half + j * 128:half + (j + 1) * 128], mb[:, j * 128:(j + 1) * 128], identb[:], ["mb", "identb"], ["pbfb%d" % p_])
                        act(mTs[p_][:].rearrange("p a b -> p (a b)"), pbf[:, half:half + 512], AF.Copy, ["pbfb%d" % p_], ["mTs%d" % p_])
                        ld("sp", MT.rearrange("(c p) t -> p c t", p=128)[:, cb4 * 4:(cb4 + 1) * 4, rows], mTs[p_][:], ["mTs%d" % p_], ["MT"])
                T.barrier_all()
            chk_stop("B1")
            with ExitStack() as st:
                mTa = sbt(st, "mTa", [128, 16, 2048], BF16)
                Wo = sbt(st, "Wo", [128, 16, 2048], BF16)
                for c4 in range(0, 16, 2):
                    ld("sp", mTa[:, c4:c4 + 2, :], MT.rearrange("(c p) t -> p c t", p=128)[:, c4:c4 + 2, :], [], ["mTa"])
                for cb4 in range(4):
                    ld("pool", Wo[:, :, cb4 * 512:(cb4 + 1) * 512], w_o[L].rearrange("(c p) n -> p c n", p=128)[:, :, cb4 * 512:(cb4 + 1) * 512], [], ["Wo"])
                mo = [sbt(st, "mo%d" % i, [128, 512], F32) for i in range(3)]
                pW = [pst(st, "pW%d" % i, [128, 512]) for i in range(4)]
                n = 0
                for i in range(16):
                    rows = slice(i * 128, (i + 1) * 128)
                    for cb4 in range(4):
                        p_ = n % 4; m_ = n % 3; n += 1
                        for kc in range(16):
                            mm(pW[p_][:], mTa[:, kc, rows], Wo[:, kc, cb4 * 512:(cb4 + 1) * 512], kc == 0, kc == 15, ["mTa", "Wo"], ["pW%d" % p_])
                        act(mo[m_][:], pW[p_][:], AF.Copy, ["pW%d" % p_], ["mo%d" % m_])
                        ld("sp", MIX[rows, cb4 * 512:(cb4 + 1) * 512], mo[m_][:], ["mo%d" % m_], ["MIX"])
                T.barrier_all()
            chk_stop("B2a")
            with ExitStack() as st:
                gbc = sbt(st, "gbc", [128, D], F32)
                lng = sbt(st, "lng", [128, D], F32)
                lnb = sbt(st, "lnb", [128, D], F32)
                ld("sp", gbc[:], MODBC[L][:, 2 * D:3 * D], [], ["gbc"])
                ld("sp", lng[:], ln1_g[L].partition_broadcast(128), [], ["lng"])
                ld("sp", lnb[:], ln1_b[L].partition_broadcast(128), [], ["lnb"])
                xr = [sbt(st, "xr%d" % i, [128, D], F32) for i in range(2)]
                mx = [sbt(st, "mx%d" % i, [128, D], F32) for i in range(2)]
                xm = [sbt(st, "xm%d" % i, [128, D], F32) for i in range(2)]
                tt = sbt(st, "lntt", [128, D], F32)
                stats = sbt(st, "lnstats", [128, 4, 6], F32)
                mv = sbt(st, "lnmv", [128, 2], F32)
                rstd = sbt(st, "lnrstd", [128, 1], F32)
                h2T = [sbt(st, "h2T%d" % i, [128, 16, 128], BF16) for i in range(2)]
                pxs = [pst(st, "pxb%d" % i, [128, 512]) for i in range(2)]
                px = [(pxs[i], "pxb%d" % i) for i in range(2)]
                if moe:
                    h2f = sbt(st, "h2f", [128, 16, 128], F32)
                    wr = sbt(st, "wr", [128, 16, 8], F32)
                    ld("sp", wr[:], router_w[0].rearrange("(c p) e -> p c e", p=128), [], ["wr"])
                    lg = sbt(st, "lg", [128, 8], F32)
                    l8 = sbt(st, "l8", [128, 8], F32)
                    dlt = sbt(st, "dlt", [128, 1], F32)
                    g12 = sbt(st, "g12", [128, 2], F32)
                    e1 = sbt(st, "e1", [128, 8], F32)
                    e2 = sbt(st, "e2", [128, 8], F32)
                    gate = [sbt(st, "gate%d" % i, [128, 8], F32) for i in range(2)]
                    pR = pst(st, "pR", [128, 512])
                for i in range(16):
                    s_ = i % 2
                    rows = slice(i * 128, (i + 1) * 128)
                    gather(xr[s_][:], xin[:, :], rw[:, 0, tcol(i):tcol(i) + 1], ["rw"], ["xr%d" % s_])
                    ld("sp", mx[s_][:], MIX[rows, :], [], ["mx%d" % s_])
                    layernorm_tile((tt, stats, mv, rstd), [(mx[s_][:, j * 512:(j + 1) * 512], "mx%d" % s_) for j in range(4)],
                                   xr[s_], gbc, lng, lnb, xm[s_], ("xr%d" % s_, None, "xm%d" % s_))
                    ld("sp", XMID[rows, :], xm[s_][:], ["xm%d" % s_], ["XMID"])
                    build_hT(xm[s_], "xm%d" % s_, h2T[s_], "h2T%d" % s_, px, 3, 4, extra=((h2f, "h2f") if moe else None))
                    ld("sp", H2T.rearrange("(c p) t -> p c t", p=128)[:, :, rows], h2T[s_][:], ["h2T%d" % s_], ["H2T"])
                    if moe:
                        for kc in range(16):
                            mm(pR[:, 0:8], h2f[:, kc, :], wr[:, kc, :], kc == 0, kc == 15, [("h2f", kc), "wr"], ["pR"])
                        T.op("dve", lambda e: e.tensor_copy(out=lg[:], in_=pR[:, 0:8]), ["pR"], ["lg"])
                        T.op("dve", lambda e: e.max(out=l8[:], in_=lg[:]), ["lg"], ["l8"])
                        T.op("dve", lambda e: e.tensor_tensor(out=dlt[:], in0=l8[:, 0:1], in1=l8[:, 1:2], op=ALU.subtract), ["l8"], ["dlt"])
                        act(g12[:, 0:1], dlt[:], AF.Sigmoid, ["dlt"], ["g12a"])
                        act(g12[:, 1:2], dlt[:], AF.Sigmoid, ["dlt"], ["g12b"], scale=-1.0)
                        T.op("dve", lambda e: e.tensor_scalar(out=e1[:], in0=lg[:], scalar1=l8[:, 0:1], scalar2=g12[:, 0:1],
                                                              op0=ALU.is_equal, op1=ALU.mult), ["lg", "l8", "g12a"], ["e1"])
                        T.op("dve", lambda e: e.tensor_scalar(out=e2[:], in0=lg[:], scalar1=l8[:, 1:2], scalar2=g12[:, 1:2],
                                                              op0=ALU.is_equal, op1=ALU.mult), ["lg", "l8", "g12b"], ["e2"])
                        T.op("dve", lambda e, s_=s_: e.tensor_tensor(out=gate[s_][:], in0=e1[:], in1=e2[:], op=ALU.add), ["e1", "e2"], ["gate%d" % s_])
                        ld("sp", GATE[rows, :], gate[s_][:], ["gate%d" % s_], ["GATE"])
                    if i == 0:
                        try:
                            chk_stop("B2b0")
                        except StopBuild:
                            T.barrier_all()
                            raise
                T.barrier_all()
            chk_stop("B")

            with ExitStack() as st:
                dff = DFE if moe else DFF
                nfc = dff // 128
                nhf = nfc // 2
                nex = 8 if moe else 1
                FB = 256
                TS = 1024
                h2 = sbt(st, "fh2", [128, 16, TS], BF16)
                hid = sbt(st, "hid", [128, nhf, TS], BF16)
                wg = [sbt(st, "wg%d" % i, [128, 16, FB], BF16) for i in range(2)]
                wu = [sbt(st, "wu%d" % i, [128, 16, FB], BF16) for i in range(2)]
                wd = [sbt(st, "wd%d" % i, [128, 4, 512], BF16) for i in range(2)]
                sg = [sbt(st, "sg%d" % i, [128, 512], BF16) for i in range(2)]
                facc = sbt(st, "facc", [128, 8, D], F32)
                gsb = sbt(st, "gsb", [128, 16, 8], F32)
                if moe:
                    ld("sp", gsb[:], GATE.rearrange("(i p) e -> p i e", p=128), [], ["gsb"])
                P = [pst(st, "pF%d" % i, [128, 512]) for i in range(8)]
                nw = 0
                nd = 0
                ng = 0
                for sc in range(2048 // TS):
                    tsl = slice(sc * TS, (sc + 1) * TS)
                    for c4 in range(0, 16, 4):
                        ld("sp", h2[:, c4:c4 + 4, :], H2T.rearrange("(c p) t -> p c t", p=128)[:, c4:c4 + 4, tsl], [], ["fh2"])
                    for ex in range(nex):
                        if moe:
                            Wg_, Wu_, Wd_ = moe_wg[0][ex], moe_wu[0][ex], moe_wd[0][ex]
                        else:
                            Wg_, Wu_, Wd_ = ffn_wg[0], ffn_wu[0], ffn_wd[0]
                        Wgv = Wg_.rearrange("(c p) n -> p c n", p=128)
                        Wuv = Wu_.rearrange("(c p) n -> p c n", p=128)
                        Wdv = Wd_.rearrange("(c p) n -> p c n", p=128)
                        for hf in range(2):
                            first = (ex == 0 and hf == 0)
                            f0 = hf * nhf
                            fcl = 0
                            while fcl < nhf:
                                nb = min(FB // 128, nhf - fcl)
                                w_ = nw % 2; nw += 1
                                c0 = (f0 + fcl) * 128
                                ld("pool", wg[w_][:, :, 0:nb * 128], Wgv[:, :, c0:c0 + nb * 128], [], ["wg%d" % w_])
                                ld("pool", wu[w_][:, :, 0:nb * 128], Wuv[:, :, c0:c0 + nb * 128], [], ["wu%d" % w_])
                                for f2 in range(nb):
                                    for tg in range(TS // 512):
                                        p_ = ng % 2; ng += 1
                                        bG, bU = P[p_ * 2], P[p_ * 2 + 1]
                                        kG, kU = "pF%d" % (p_ * 2), "pF%d" % (p_ * 2 + 1)
                                        tk = slice(tg * 512, (tg + 1) * 512)
                                        for kc in range(16):
                                            mm(bG[:], wg[w_][:, kc, f2 * 128:(f2 + 1) * 128], h2[:, kc, tk], kc == 0, kc == 15,
                                               ["wg%d" % w_, "fh2"], [kG])
                                        for kc in range(16):
                                            mm(bU[:], wu[w_][:, kc, f2 * 128:(f2 + 1) * 128], h2[:, kc, tk], kc == 0, kc == 15,
                                               ["wu%d" % w_, "fh2"], [kU])
                                        act(sg[p_][:], bG[:], AF.Silu, [kG], ["sg%d" % p_])
                                        T.op("dve", lambda e, p_=p_, fi=fcl + f2, bU=bU, tk=tk: e.tensor_tensor(
                                            out=hid[:, fi, tk], in0=bU[:], in1=sg[p_][:], op=ALU.mult), [kU, "sg%d" % p_], ["hid"])
                                fcl += nb
                            for q in range(4):
                                cs = slice(q * 512, (q + 1) * 512)
                                fi = 0
                                while fi < nhf:
                                    nb = min(4, nhf - fi)
                                    d_ = nd % 2; nd += 1
                                    ld("pool", wd[d_][:, 0:nb, :], Wdv[:, f0 + fi:f0 + fi + nb, cs], [], ["wd%d" % d_])
                                    for f4 in range(nb):
                                        fc = fi + f4
                                        for ti in range(TS // 128):
                                            mm(P[ti][:], hid[:, fc, ti * 128:(ti + 1) * 128], wd[d_][:, f4, :],
                                               fc == 0, fc == nhf - 1, ["hid", "wd%d" % d_], ["pF%d" % ti])
                                    fi += nb
                                for ti in range(TS // 128):
                                    gi_ = sc * (TS // 128) + ti
                                    fk = ("facc", ti, q)
                                    if first and not moe:
                                        T.op("dve", lambda e, ti=ti, cs=cs: e.tensor_copy(out=facc[:, ti, cs], in_=P[ti][:]), ["pF%d" % ti], [fk])
                                    elif first:
                                        T.op("dve", lambda e, ti=ti, cs=cs, gi_=gi_, ex=ex: e.tensor_scalar(
                                            out=facc[:, ti, cs], in0=P[ti][:], scalar1=gsb[:, gi_, ex:ex + 1], scalar2=None, op0=ALU.mult),
                                            ["pF%d" % ti, "gsb"], [fk])
                                    elif not moe:
                                        T.op("dve", lambda e, ti=ti, cs=cs: e.tensor_tensor(out=facc[:, ti, cs], in0=P[ti][:], in1=facc[:, ti, cs], op=ALU.add),
                                             ["pF%d" % ti, fk], [fk])
                                    else:
                                        T.op("dve", lambda e, ti=ti, cs=cs, gi_=gi_, ex=ex: e.scalar_tensor_tensor(
                                            out=facc[:, ti, cs], in0=P[ti][:], scalar=gsb[:, gi_, ex:ex + 1], in1=facc[:, ti, cs],
                                            op0=ALU.mult, op1=ALU.add), ["pF%d" % ti, "gsb", fk], [fk])
                    for ti in range(TS // 128):
                        i = sc * (TS // 128) + ti
                        ld("sp", FOUT[i * 128:(i + 1) * 128, :], facc[:, ti, :], [("facc", ti, q) for q in range(4)], ["FOUT"])
                T.barrier_all()
            with ExitStack() as st:
                gbc = sbt(st, "gbc", [128, D], F32)
                lng = sbt(st, "lng", [128, D], F32)
                lnb = sbt(st, "lnb", [128, D], F32)
                ld("sp", gbc[:], MODBC[L][:, 5 * D:6 * D], [], ["gbc"])
                ld("sp", lng[:], ln2_g[L].partition_broadcast(128), [], ["lng"])
                ld("sp", lnb[:], ln2_b[L].partition_broadcast(128), [], ["lnb"])
                xm = [sbt(st, "fxm%d" % i, [128, D], F32) for i in range(2)]
                ff = [sbt(st, "fff%d" % i, [128, D], F32) for i in range(2)]
                xo = [sbt(st, "fxo%d" % i, [128, D], F32) for i in range(2)]
                tt = sbt(st, "lntt", [128, D], F32)
                stats = sbt(st, "lnstats", [128, 4, 6], F32)
                mv = sbt(st, "lnmv", [128, 2], F32)
                rstd = sbt(st, "lnrstd", [128, 1], F32)
                for i in range(16):
                    x_ = i % 2
                    rows = slice(i * 128, (i + 1) * 128)
                    ld("sp", xm[x_][:], XMID[rows, :], [], ["fxm%d" % x_])
                    ld("sp", ff[x_][:], FOUT[rows, :], [], ["fff%d" % x_])
                    layernorm_tile((tt, stats, mv, rstd), [(ff[x_][:, j * 512:(j + 1) * 512], "fff%d" % x_) for j in range(4)],
                                   xm[x_], gbc, lng, lnb, xo[x_], ("fxm%d" % x_, None, "fxo%d" % x_))
                    orow = ch * 2048 + i * 128
                    ld("sp", xout[orow:orow + 128, :], xo[x_][:], ["fxo%d" % x_], ["XOUT"])
                T.barrier_all()


        try:
            layer(0, xb, 2, rw1, k_posrow, poscol, X1, False)
            layer(1, X1, 1, rw2, q2_posrow, q2pc, out, True)
        except StopBuild:
            pass
        T.barrier_all()
        build.ninst = T.ninst
        build.nops = T.nops
        build.lines = T.lines
    return nc


def host_tables(parity):
    ident = np.eye(128, dtype=np.float32)
    posrow = np.tile(np.arange(S, dtype=np.float32)[None, :], (128, 1))
    poscol = (np.arange(NT, dtype=np.float32)[None, :] * 128 + np.arange(128, dtype=np.float32)[:, None]).astype(np.float32)
    inv = np.concatenate([THETA ** (-np.arange(0, rd, 2, dtype=np.float32) / rd) for rd in (64, 32, 16)]).astype(np.float32)
    k_inv = np.tile(inv[None, :], (128, 1)).astype(np.float32)
    own_tiles = 2 * np.arange(16) + parity
    own_rows = (own_tiles[None, :] * 128 + np.arange(128)[:, None]).astype(np.int32)
    q2_posrow = np.tile(own_rows.T.reshape(-1).astype(np.float32)[None, :], (128, 1))
    q2_poscol = own_rows.astype(np.float32)
    r1 = (np.arange(NT)[None, :] * 128 + np.arange(128)[:, None]).astype(np.int32)
    rows1 = np.stack([r1, r1 + 1, r1 + 2]).astype(np.int32)
    rows2 = np.stack([own_rows, own_rows + 1, own_rows + 2]).astype(np.int32)
    return dict(k_ident=ident, k_posrow=posrow, k_poscol=poscol, k_inv=k_inv, q2_posrow=q2_posrow,
                q2_poscol=q2_poscol, rows1=rows1, rows2=rows2), own_rows


def make_in_map(inputs, core):
    b, p = core // 2, core % 2
    tabs, own_rows = host_tables(p)
    m = dict(tabs)
    m["xb"] = np.ascontiguousarray(inputs["x"][b])
    m["c_col"] = np.ascontiguousarray(inputs["c"][b].reshape(16, 128).T)
    m["pos_col"] = np.ascontiguousarray(inputs["positions"][b].reshape(NT, 128).T.astype(np.int32))
    for k in ("ada_w", "ada_b", "ln1_g", "ln1_b", "ln2_g", "ln2_b", "w_in", "mla_q_norm", "mla_kv_norm", "w_uq", "w_ukv",
              "conv_w", "conv_b", "w_branch_a", "w_branch_b", "w_branch_c", "w_o", "ffn_w_gate", "ffn_w_up", "ffn_w_down",
              "router_w", "moe_w_gate", "moe_w_up", "moe_w_down"):
        m[k] = inputs[k]
    return m, own_rows


def kernel(**inputs):
    inputs = {k: np.asarray(v) for k, v in inputs.items()}
    nc = build()
    maps = []
    owns = []
    for core in range(8):
        m, own = make_in_map(inputs, core)
        maps.append(m)
        owns.append(own)
    res = run_bass_kernel_spmd(nc, maps, core_ids=list(range(8)))
    outp = np.zeros((4, S, D), dtype=np.float32)
    for core in range(8):
        b = core // 2
        o = np.asarray(res.results[core]["out"]).reshape(16, 128, D)
        rows = owns[core].T
        for i in range(16):
            outp[b, rows[i]] = o[i]
    return outp
```
